# Optimizing a Trainium2 kernel written in Bass

```python
import jax, jax.numpy as jnp
from jax import lax
import numpy as np

D_MODEL = 1024
BATCH = 2
SEQ = 16384
DEPTH = 4

HEAD_DIM = 64
BLOCK = 128
A_Q_HEADS = 8
A_KV_HEADS = 2
A_WINDOW = 128
B_Q_HEADS = 8
B_KV_HEADS = 2
CMP_LEN = 32
CMP_STRIDE = 16
CMP_HIDDEN = 128
SLC_LEN = 64
N_SLC = 16
N_LOCAL_SLC = 2
B_WINDOW = 512
D_FF = 2816
CONV_WIDTH = 3
ROPE_THETA = 10000.0
EPS = 1e-6
TINY = 1e-30

A_WIDTH = A_Q_HEADS * HEAD_DIM
A_KV_WIDTH = A_KV_HEADS * HEAD_DIM
B_WIDTH = B_Q_HEADS * HEAD_DIM
B_KV_WIDTH = B_KV_HEADS * HEAD_DIM
N_NSA_GATES = 3 * B_Q_HEADS
IN_SIZES = (A_WIDTH, A_KV_WIDTH, A_KV_WIDTH,
            B_WIDTH, B_KV_WIDTH, B_KV_WIDTH, B_KV_WIDTH, B_KV_WIDTH, B_KV_WIDTH, B_KV_WIDTH,
            N_NSA_GATES, D_MODEL, D_MODEL)
IN_COLS = sum(IN_SIZES)

kernel_name = 'hybrid_swa_sink_nsa_convffn'


def rms_norm(x, gain):
    xf = x.astype(jnp.float32)
    y = xf * lax.rsqrt(jnp.mean(xf * xf, axis=-1, keepdims=True) + EPS)
    return (y * gain.astype(jnp.float32)).astype(x.dtype)


def apply_rope(x, positions):
    half = HEAD_DIM // 2
    inv_freq = ROPE_THETA ** (-jnp.arange(half, dtype=jnp.float32) / half)
    ang = positions.astype(jnp.float32)[..., None] * inv_freq
    cos = jnp.cos(ang)[:, :, None, :]
    sin = jnp.sin(ang)[:, :, None, :]
    xf = x.astype(jnp.float32)
    x1, x2 = xf[..., :half], xf[..., half:]
    return jnp.concatenate([x1 * cos - x2 * sin, x2 * cos + x1 * sin], axis=-1).astype(x.dtype)


def masked_softmax(s, mask):
    s = jnp.where(mask, s, -jnp.inf)
    m = jnp.max(s, axis=-1, keepdims=True)
    m = jnp.where(jnp.isfinite(m), m, 0.0)
    p = jnp.exp(s - m)
    return p / jnp.maximum(jnp.sum(p, axis=-1, keepdims=True), TINY)


def band_context(t, n_prev):
    nb = t.shape[1]
    pad = [(0, 0)] * t.ndim
    pad[1] = (n_prev, 0)
    tp = jnp.pad(t, pad)
    return jnp.concatenate([tp[:, j:j + nb] for j in range(n_prev + 1)], axis=2)


def swa_sink_attention(q, k, v, sinks):
    b, s, g, r, d = q.shape
    nb = s // BLOCK
    n_prev = -(-A_WINDOW // BLOCK)
    qb = q.reshape(b, nb, BLOCK, g, r, d)
    kc = band_context(k.reshape(b, nb, BLOCK, g, d), n_prev)
    vc = band_context(v.reshape(b, nb, BLOCK, g, d), n_prev)
    scores = jnp.einsum('bnqgrd,bnkgd->bngrqk', qb, kc).astype(jnp.float32) * (d ** -0.5)
    blk = jnp.arange(nb)[:, None, None] * BLOCK
    qpos = blk + jnp.arange(BLOCK)[None, :, None]
    kpos = blk - n_prev * BLOCK + jnp.arange((n_prev + 1) * BLOCK)[None, None, :]
    rel = qpos - kpos
    mask = (rel >= 0) & (rel < A_WINDOW) & (kpos >= 0)
    scores = jnp.where(mask[None, :, None, None], scores, -jnp.inf)
    sink = sinks.astype(jnp.float32).reshape(1, 1, g, r, 1, 1)
    m = jnp.maximum(jnp.max(scores, axis=-1, keepdims=True), sink)
    p = jnp.exp(scores - m)
    p = p / (jnp.sum(p, axis=-1, keepdims=True) + jnp.exp(sink - m))
    o = jnp.einsum('bngrqk,bnkgd->bnqgrd', p.astype(v.dtype), vc)
    return o.reshape(b, s, g * r * d)


def compress(t, pos_emb, w1, b1, w2):
    b, s, g, d = t.shape
    ratio = CMP_LEN // CMP_STRIDE
    chunks = t.reshape(b, s // CMP_STRIDE, CMP_STRIDE, g, d)
    nc = s // CMP_STRIDE - ratio + 1
    blocks = jnp.concatenate([chunks[:, j:j + nc] for j in range(ratio)], axis=2)
    blocks = blocks + pos_emb[None, None, :, None, :]
    flat = blocks.transpose(0, 1, 3, 2, 4).reshape(b, nc, g, CMP_LEN * d)
    hid = jax.nn.gelu(flat @ w1 + b1, approximate=True)
    return hid @ w2


def nsa_attention(q_raw, q_rot, k_cmp, v_cmp, k_slc, v_slc, k_win, v_win, gates):
    b, s, g, r, d = q_rot.shape
    nb = s // BLOCK
    nc = k_cmp.shape[1]
    ns = s // SLC_LEN
    n_sel = min(N_SLC, ns)
    scale = d ** -0.5
    ratio_c = CMP_LEN // CMP_STRIDE
    ratio_s = SLC_LEN // CMP_STRIDE
    ks_blocks = k_slc.reshape(b, ns, SLC_LEN, g, d).transpose(0, 3, 1, 2, 4)
    vs_blocks = v_slc.reshape(b, ns, SLC_LEN, g, d).transpose(0, 3, 1, 2, 4)
    kw_pad = jnp.pad(k_win, ((0, 0), (B_WINDOW, 0), (0, 0), (0, 0)))
    vw_pad = jnp.pad(v_win, ((0, 0), (B_WINDOW, 0), (0, 0), (0, 0)))
    cmp_end = jnp.arange(nc) * CMP_STRIDE + CMP_LEN - 1
    blk_ids = jnp.arange(ns)
    gather = jax.vmap(jax.vmap(lambda blocks, ix: blocks[ix]))

    def one_block(i):
        start = i * BLOCK
        t = start + jnp.arange(BLOCK)
        qr = lax.dynamic_slice_in_dim(q_rot, start, BLOCK, axis=1)
        qn = lax.dynamic_slice_in_dim(q_raw, start, BLOCK, axis=1)
        gt = lax.dynamic_slice_in_dim(gates, start, BLOCK, axis=1)
        s_c = jnp.einsum('bqgrd,bcgd->bgrqc', qn, k_cmp).astype(jnp.float32) * scale
        p_c = masked_softmax(s_c, cmp_end[None, :] <= t[:, None])
        o_c = jnp.einsum('bgrqc,bcgd->bqgrd', p_c.astype(v_cmp.dtype), v_cmp)
        imp = jnp.sum(p_c, axis=2)
        imp = jnp.pad(imp, ((0, 0), (0, 0), (0, 0), (ratio_c - 1, ratio_s * ns - nc)))
        imp_s = jnp.zeros(imp.shape[:-1] + (ns,), jnp.float32)
        for m in range(ratio_s):
            for n in range(ratio_c):
                off = m - n + ratio_c - 1
                imp_s = imp_s + imp[..., off:off + ratio_s * ns:ratio_s]
        cur = t // SLC_LEN
        valid = blk_ids[None, :] <= cur[:, None]
        forced = (blk_ids[None, :] == 0) | (valid & (blk_ids[None, :] > cur[:, None] - N_LOCAL_SLC))
        imp_s = jnp.where(forced, jnp.inf, jnp.where(valid, imp_s, -jnp.inf))
        _, idx = lax.top_k(imp_s, n_sel)
        k_sel = gather(ks_blocks, idx)
        v_sel = gather(vs_blocks, idx)
        s_s = jnp.einsum('bqgrd,bgqkld->bgrqkl', qr, k_sel).astype(jnp.float32) * scale
        tok = idx[..., None] * SLC_LEN + jnp.arange(SLC_LEN)
        mask_s = (tok <= t[None, None, :, None, None]).reshape(b, g, 1, BLOCK, n_sel * SLC_LEN)
        p_s = masked_softmax(s_s.reshape(b, g, r, BLOCK, n_sel * SLC_LEN), mask_s)
        o_s = jnp.einsum('bgrqn,bgqnd->bqgrd', p_s.astype(v_slc.dtype),
                         v_sel.reshape(b, g, BLOCK, n_sel * SLC_LEN, d))
        kwin = lax.dynamic_slice_in_dim(kw_pad, start, BLOCK + B_WINDOW, axis=1)
        vwin = lax.dynamic_slice_in_dim(vw_pad, start, BLOCK + B_WINDOW, axis=1)
        kpos = start - B_WINDOW + jnp.arange(BLOCK + B_WINDOW)
        rel = t[:, None] - kpos[None, :]
        mask_w = (rel >= 0) & (rel < B_WINDOW) & (kpos[None, :] >= 0)
        s_w = jnp.einsum('bqgrd,bkgd->bgrqk', qr, kwin).astype(jnp.float32) * scale
        p_w = masked_softmax(s_w, mask_w)
        o_w = jnp.einsum('bgrqk,bkgd->bqgrd', p_w.astype(v_win.dtype), vwin)
        return gt[..., 0:1] * o_c + gt[..., 1:2] * o_s + gt[..., 2:3] * o_w

    out = lax.map(one_block, jnp.arange(nb))
    return out.transpose(1, 0, 2, 3, 4, 5).reshape(b, s, g * r * d)


def conv_ffn(h, w_up, conv_w, conv_b, w_down):
    u = h @ w_up
    s = u.shape[1]
    up = jnp.pad(u, ((0, 0), (CONV_WIDTH - 1, 0), (0, 0)))
    c = conv_b
    for k in range(CONV_WIDTH):
        c = c + conv_w[k] * up[:, k:k + s]
    a, val = jnp.split(c, 2, axis=-1)
    return (jax.nn.gelu(a, approximate=True) * val) @ w_down


def hybrid_layer(x, positions, attn_pre_gain, attn_post_gain, ffn_pre_gain, ffn_post_gain,
                 w_in, attn_sinks, cmp_pos_emb, cmp_w1, cmp_b1, cmp_w2,
                 w_branch_a, w_branch_b, w_out, ffn_w_up, ffn_conv_w, ffn_conv_b, ffn_w_down):
    b, s, _ = x.shape
    hd = HEAD_DIM
    h = rms_norm(x, attn_pre_gain)
    u = h @ w_in
    offsets = np.cumsum(np.array(IN_SIZES))[:-1].tolist()
    (qa, ka, va, qb, kcb, vcb, ksb, vsb, kwb, vwb, g_nsa, g_a, g_b) = jnp.split(u, offsets, axis=-1)
    ra = A_Q_HEADS // A_KV_HEADS
    qa = apply_rope(qa.reshape(b, s, A_Q_HEADS, hd), positions).reshape(b, s, A_KV_HEADS, ra, hd)
    ka = apply_rope(ka.reshape(b, s, A_KV_HEADS, hd), positions)
    va = va.reshape(b, s, A_KV_HEADS, hd)
    o_a = swa_sink_attention(qa, ka, va, attn_sinks)
    rb = B_Q_HEADS // B_KV_HEADS
    qb = qb.reshape(b, s, B_Q_HEADS, hd)
    qb_rot = apply_rope(qb, positions).reshape(b, s, B_KV_HEADS, rb, hd)
    qb_raw = qb.reshape(b, s, B_KV_HEADS, rb, hd)
    kc = compress(kcb.reshape(b, s, B_KV_HEADS, hd), cmp_pos_emb[0], cmp_w1[0], cmp_b1[0], cmp_w2[0])
    vc = compress(vcb.reshape(b, s, B_KV_HEADS, hd), cmp_pos_emb[1], cmp_w1[1], cmp_b1[1], cmp_w2[1])
    ks = apply_rope(ksb.reshape(b, s, B_KV_HEADS, hd), positions)
    kw = apply_rope(kwb.reshape(b, s, B_KV_HEADS, hd), positions)
    vs = vsb.reshape(b, s, B_KV_HEADS, hd)
    vw = vwb.reshape(b, s, B_KV_HEADS, hd)
    gates = jax.nn.sigmoid(g_nsa).reshape(b, s, B_KV_HEADS, rb, 3)
    o_b = nsa_attention(qb_raw, qb_rot, kc, vc, ks, vs, kw, vw, gates)
    mixed = jax.nn.sigmoid(g_a) * (o_a @ w_branch_a) + jax.nn.sigmoid(g_b) * (o_b @ w_branch_b)
    x = x + rms_norm(mixed @ w_out, attn_post_gain)
    h = rms_norm(x, ffn_pre_gain)
    x = x + rms_norm(conv_ffn(h, ffn_w_up, ffn_conv_w, ffn_conv_b, ffn_w_down), ffn_post_gain)
    return x


def setup_inputs(seed: int = 0) -> dict:
    key = jax.random.key(seed)
    ks = jax.random.split(key, 20)
    f32 = jnp.float32

    def nrm(k, shape, scale):
        return scale * jax.random.normal(k, shape, f32)

    x = nrm(ks[0], (BATCH, SEQ, D_MODEL), 1.0)
    positions = jnp.broadcast_to(jnp.arange(SEQ, dtype=jnp.int32)[None, :], (BATCH, SEQ))
    return {
        'x': x,
        'positions': positions,
        'attn_pre_gain': 1.0 + nrm(ks[1], (DEPTH, D_MODEL), 0.05),
        'attn_post_gain': 1.0 + nrm(ks[2], (DEPTH, D_MODEL), 0.05),
        'ffn_pre_gain': 1.0 + nrm(ks[3], (DEPTH, D_MODEL), 0.05),
        'ffn_post_gain': 1.0 + nrm(ks[4], (DEPTH, D_MODEL), 0.05),
        'w_in': nrm(ks[5], (DEPTH, D_MODEL, IN_COLS), D_MODEL ** -0.5),
        'attn_sinks': nrm(ks[6], (DEPTH, A_Q_HEADS), 1.0),
        'cmp_pos_emb': nrm(ks[7], (DEPTH, 2, CMP_LEN, HEAD_DIM), 0.1),
        'cmp_w1': nrm(ks[8], (DEPTH, 2, CMP_LEN * HEAD_DIM, CMP_HIDDEN), (CMP_LEN * HEAD_DIM) ** -0.5),
        'cmp_b1': nrm(ks[9], (DEPTH, 2, CMP_HIDDEN), 0.01),
        'cmp_w2': nrm(ks[10], (DEPTH, 2, CMP_HIDDEN, HEAD_DIM), CMP_HIDDEN ** -0.5),
        'w_branch_a': nrm(ks[11], (DEPTH, A_WIDTH, D_MODEL), A_WIDTH ** -0.5),
        'w_branch_b': nrm(ks[12], (DEPTH, B_WIDTH, D_MODEL), B_WIDTH ** -0.5),
        'w_out': nrm(ks[13], (DEPTH, D_MODEL, D_MODEL), D_MODEL ** -0.5),
        'ffn_w_up': nrm(ks[14], (DEPTH, D_MODEL, 2 * D_FF), D_MODEL ** -0.5),
        'ffn_conv_w': nrm(ks[15], (DEPTH, CONV_WIDTH, 2 * D_FF), CONV_WIDTH ** -0.5),
        'ffn_conv_b': nrm(ks[16], (DEPTH, 2 * D_FF), 0.01),
        'ffn_w_down': nrm(ks[17], (DEPTH, D_FF, D_MODEL), D_FF ** -0.5),
    }


def reference(x, positions, attn_pre_gain, attn_post_gain, ffn_pre_gain, ffn_post_gain,
              w_in, attn_sinks, cmp_pos_emb, cmp_w1, cmp_b1, cmp_w2,
              w_branch_a, w_branch_b, w_out, ffn_w_up, ffn_conv_w, ffn_conv_b, ffn_w_down):
    for l in range(DEPTH):
        x = hybrid_layer(x, positions, attn_pre_gain[l], attn_post_gain[l], ffn_pre_gain[l],
                         ffn_post_gain[l], w_in[l], attn_sinks[l], cmp_pos_emb[l], cmp_w1[l],
                         cmp_b1[l], cmp_w2[l], w_branch_a[l], w_branch_b[l], w_out[l],
                         ffn_w_up[l], ffn_conv_w[l], ffn_conv_b[l], ffn_w_down[l])
    return x
```

```python
import numpy as np
from contextlib import ExitStack
import concourse.bass as bass
import concourse.mybir as mybir
from concourse.bass_utils import run_bass_kernel_spmd

F32 = mybir.dt.float32
BF16 = mybir.dt.bfloat16
I32 = mybir.dt.int32
ALU = mybir.AluOpType
AF = mybir.ActivationFunctionType

NCORES = 8
D = 1024
SEQ = 16384
TOK = 4096
NB = 32
INC = 4120
DFF = 2816
EPS = 1e-6


class Buf:
    __slots__ = ("name", "last_w", "readers")

    def __init__(self, name):
        self.name = name
        self.last_w = None
        self.readers = []


class Op:
    __slots__ = ("eng", "fn", "deps", "signal", "sig", "semkey", "dma", "idx")


class Prog:
    def __init__(self, nc):
        self.nc = nc
        self.ops = []
        self.stack = ExitStack()
        self.n_dma = {}
        self.out_dma_keys = set()

    def sb(self, name, shape, dt):
        return self.stack.enter_context(self.nc.sbuf_tensor(name, list(shape), dt))

    def ps(self, name, shape, dt=F32):
        return self.stack.enter_context(self.nc.psum_tensor(name, list(shape), dt))

    def add(self, eng, fn, R=(), W=(), dma=None):
        op = Op()
        op.eng = eng
        op.fn = fn
        op.signal = False
        op.sig = None
        op.dma = dma
        op.semkey = None
        op.idx = len(self.ops)
        deps = {}
        for b in R:
            if b.last_w is not None:
                deps[b.last_w.idx] = b.last_w
        for b in W:
            if b.last_w is not None:
                deps[b.last_w.idx] = b.last_w
            for r in b.readers:
                deps[r.idx] = r
        op.deps = []
        for d in deps.values():
            if d.eng == "pe" and eng == "pe" and d.dma is None and dma is None:
                continue
            d.signal = True
            op.deps.append(d)
        for b in R:
            b.readers.append(op)
        for b in W:
            b.last_w = op
            b.readers = []
        if dma is not None:
            op.signal = True
            n = self.n_dma.get(dma, 0) + 1
            self.n_dma[dma] = n
            op.semkey = "dma_" + dma
            op.sig = 16 * n
        self.ops.append(op)
        return op

    def mm(self, out, lhsT, rhs, start=True, stop=True, R=(), W=(), **kw):
        return self.add("pe", lambda e: e.matmul(out, lhsT, rhs, start=start, stop=stop, **kw), R, W)

    def tr(self, out, in_, ident, R=(), W=()):
        return self.add("pe", lambda e: e.transpose(out, in_, ident), R, W)

    def act(self, out, in_, func, R=(), W=(), **kw):
        return self.add("act", lambda e: e.activation(out, in_, func, **kw), R, W)

    def v(self, eng, name, *args, R=(), W=(), **kw):
        return self.add(eng, lambda e: getattr(e, name)(*args, **kw), R, W)

    def dma(self, out, in_, key, R=(), W=(), eng="sp", is_out=False, **kw):
        if is_out:
            self.out_dma_keys.add(key)
        return self.add(eng, lambda e: e.dma_start(out=out, in_=in_, **kw), R, W, dma=key)

    def emit(self):
        nc = self.nc
        engs = ["pe", "act", "dve", "pool", "sp"]
        cnt = {e: 0 for e in engs}
        for op in self.ops:
            if op.dma is None and op.signal:
                cnt[op.eng] += 1
                op.sig = cnt[op.eng]
                op.semkey = "eng_" + op.eng
        semkeys = sorted({op.semkey for op in self.ops if op.semkey is not None})
        sems = {k: self.stack.enter_context(nc.semaphore(k)) for k in semkeys}
        per = {e: [op for op in self.ops if op.eng == e] for e in engs}
        final = [(sems["dma_" + k], 16 * self.n_dma[k]) for k in sorted(self.out_dma_keys)]
        block = self.stack.enter_context(nc.Block())

        def run(e, name):
            known = {}
            for op in per[name]:
                need = {}
                for d in op.deps:
                    if known.get(d.semkey, 0) < d.sig:
                        need[d.semkey] = max(need.get(d.semkey, 0), d.sig)
                for k, val in need.items():
                    e.wait_ge(sems[k], val)
                    known[k] = val
                ins = op.fn(e)
                if op.signal:
                    ins.then_inc(sems[op.semkey], 16 if op.dma is not None else 1)
            if name == "sp":
                for s, val in final:
                    e.wait_ge(s, val)

        @block.tensor
        def _(e):
            run(e, "pe")

        @block.scalar
        def _(e):
            run(e, "act")

        @block.vector
        def _(e):
            run(e, "dve")

        @block.gpsimd
        def _(e):
            run(e, "pool")

        @block.sync
        def _(e):
            run(e, "sp")

    def close(self):
        self.stack.close()


class Rot:
    def __init__(self, items):
        self.items = items
        self.i = 0

    def next(self):
        it = self.items[self.i % len(self.items)]
        self.i += 1
        return it


def bc(ap, shape, axis):
    return ap.unsqueeze(axis).broadcast_to(list(shape))


A_TCH = ([(i, 128 * i, True, None) for i in range(4)]
         + [(4, 512, True, None)]
         + [(5 + i, 768 + 128 * i, True, 9 + i) for i in range(4)]
         + [(13, 1280, False, None), (14, 1408, False, None)]
         + [(15, 1536, True, None), (16, 1792, True, None)])
A_ROT_COLS = [(0, 512), (512, 128), (768, 512), (1536, 128), (1792, 128)]
A_VCH = [(640, 128), (1664, 128), (1920, 152), (2072, 512), (2584, 512), (3096, 512), (3608, 512)]
TWO_PI = 2.0 * np.pi
C1 = 6.28125
C2 = TWO_PI - C1


def build_A(ngroups=TOK // 512, do_rope=True, do_w=True, lvl=3, oeng="pool", addeng="pool", tch=None):
    nc = bass.Bass("TRN2", target_bir_lowering=False)
    x = nc.dram_tensor("x", [TOK, D], F32, kind="ExternalInput").ap()
    pos = nc.dram_tensor("pos", [1, TOK], I32, kind="ExternalInput").ap()
    w_in = nc.dram_tensor("w_in", [D, INC], F32, kind="ExternalInput").ap()
    gain = nc.dram_tensor("gain", [128, 8], F32, kind="ExternalInput").ap()
    invf = nc.dram_tensor("invf", [128, 1], F32, kind="ExternalInput").ap()
    identd = nc.dram_tensor("ident", [128, 128], F32, kind="ExternalInput").ap()
    OT = nc.dram_tensor("OT", [17, 128, TOK], BF16, kind="ExternalOutput").ap()
    OV = nc.dram_tensor("OV", [TOK, 390], BF16, kind="ExternalOutput").ap()
    OG = nc.dram_tensor("OG", [TOK, 2072], F32, kind="ExternalOutput").ap()
    p = Prog(nc)

    wb = p.sb("wb", [128, 8, INC], BF16); b_wb = Buf("wb")
    rotmap = {}
    off = 0
    for c0, n in A_ROT_COLS:
        rotmap[c0] = off
        off += n
    NR = off
    wr = p.sb("wr", [128, 8, NR], BF16); b_wr = Buf("wr")
    sinT = p.sb("sinT", [128, TOK], F32); b_sin = Buf("sin")
    cosT = p.sb("cosT", [128, TOK], F32); b_cos = Buf("cos")
    gain_sb = p.sb("gain_sb", [128, 8], F32); b_gain = Buf("gain")
    invf_sb = p.sb("invf_sb", [128, 1], F32); b_invf = Buf("invf")
    identf = p.sb("identf", [128, 128], F32); b_identf = Buf("identf")
    ident = p.sb("identb", [128, 128], BF16); b_ident = Buf("ident")

    p.dma(gain_sb[:], gain[:, :], "gain", W=[b_gain])
    p.dma(invf_sb[:], invf[:, :], "invf", W=[b_invf])
    p.dma(identf[:], identd[:, :], "identf", W=[b_identf])
    p.v("dve", "tensor_copy", ident[:], identf[:], R=[b_identf], W=[b_ident])

    SW = 1030
    stg = Rot([(p.sb(f"stg{i}", [128, SW], F32), Buf(f"stg{i}")) for i in range(2)])
    ci = 0
    for k in range(8 if do_w else 0):
        for q in range(4):
            st, bst = stg.next()
            p.dma(st[:], w_in[k * 128:(k + 1) * 128, q * SW:(q + 1) * SW], bst.name, W=[bst])
            eng = ["act", "dve", "pool"][ci % 3]
            ci += 1
            if eng == "act":
                p.act(wb[:, k, q * SW:(q + 1) * SW], st[:], AF.Copy, R=[bst], W=[b_wb])
            else:
                p.v(eng, "tensor_copy", wb[:, k, q * SW:(q + 1) * SW], st[:], R=[bst], W=[b_wb])
    for k in range(8):
        for c0, n in A_ROT_COLS:
            src = wb[:, k, c0:c0 + n].rearrange("p (h t d) -> p h t d", t=2, d=32)
            dst = wr[:, k, rotmap[c0]:rotmap[c0] + n].rearrange("p (h t d) -> p h t d", t=2, d=32)
            p.v("pool", "tensor_scalar", dst[:, :, 0, :], src[:, :, 1, :], -1.0, None, ALU.mult, R=[b_wb], W=[b_wr])
            p.v("pool", "tensor_copy", dst[:, :, 1, :], src[:, :, 0, :], R=[b_wb], W=[b_wr])

    RC = 512
    posi = p.sb("posi", [128, RC], I32); b_posi = Buf("posi")
    ang = p.sb("ang", [128, RC], F32); b_ang = Buf("ang")
    ki = p.sb("ki", [128, RC], I32); b_ki = Buf("ki")
    kf = p.sb("kf", [128, RC], F32); b_kf = Buf("kf")
    rr = p.sb("rr", [128, RC], F32); b_rr = Buf("rr")
    mk = p.sb("mk", [128, RC], F32); b_mk = Buf("mk")
    for c in range(TOK // RC if do_rope else 0):
        sl = slice(c * RC, (c + 1) * RC)
        p.dma(posi[:], pos[0:1, sl].broadcast_to([128, RC]), "posi", W=[b_posi])
        p.v("dve", "tensor_copy", ang[:], posi[:], R=[b_posi], W=[b_ang])
        p.v("dve", "tensor_scalar", ang[:], ang[:], invf_sb[:, 0:1], None, ALU.mult, R=[b_ang, b_invf], W=[b_ang])
        for tab, b_tab, shift in ((sinT, b_sin, 0.0), (cosT, b_cos, 0.5 * np.pi)):
            p.v("dve", "tensor_scalar", ki[:], ang[:], float(shift), 1.0 / TWO_PI, ALU.add, ALU.mult, R=[b_ang], W=[b_ki])
            p.v("dve", "tensor_copy", kf[:], ki[:], R=[b_ki], W=[b_kf])
            p.v("dve", "scalar_tensor_tensor", rr[:], kf[:], -C1, ang[:], ALU.mult, ALU.add, R=[b_kf, b_ang], W=[b_rr])
            p.v("dve", "scalar_tensor_tensor", rr[:], kf[:], -C2, rr[:], ALU.mult, ALU.add, R=[b_kf, b_rr], W=[b_rr])
            p.v("dve", "tensor_scalar", rr[:], rr[:], float(shift), None, ALU.add, R=[b_rr], W=[b_rr])
            p.v("dve", "tensor_scalar", mk[:], rr[:], -np.pi, TWO_PI, ALU.is_lt, ALU.mult, R=[b_rr], W=[b_mk])
            p.v("dve", "tensor_tensor", rr[:], rr[:], mk[:], ALU.add, R=[b_rr, b_mk], W=[b_rr])
            p.v("dve", "tensor_scalar", mk[:], rr[:], np.pi, -TWO_PI, ALU.is_gt, ALU.mult, R=[b_rr], W=[b_mk])
            p.v("dve", "tensor_tensor", rr[:], rr[:], mk[:], ALU.add, R=[b_rr, b_mk], W=[b_rr])
            p.v("dve", "tensor_scalar", rr[:], rr[:], 3.141592, -3.141592, ALU.min, ALU.max, R=[b_rr], W=[b_rr])
            p.act(tab[:, sl], rr[:], AF.Sin, R=[b_rr], W=[b_tab])

    xts = Rot([(p.sb(f"xt{i}", [128, D], F32), Buf(f"xt{i}")) for i in range(2)])
    xns = Rot([(p.sb(f"xn{i}", [128, D], BF16), Buf(f"xn{i}")) for i in range(2)])
    sts = Rot([(p.sb(f"st{i}", [128, 4], F32), Buf(f"stat{i}")) for i in range(2)])
    hT = p.sb("hT", [128, 8, 512], BF16); b_hT = Buf("hT")
    pst = p.ps("pst", [128, 8, 128], BF16); b_pst = Buf("pst")
    psU = Rot([(p.ps(f"psU{i}", [128, 512]), Buf(f"psU{i}")) for i in range(2)])
    psR = Rot([(p.ps(f"psR{i}", [128, 512]), Buf(f"psR{i}")) for i in range(2)])
    psV = Rot([(p.ps(f"psV{i}", [128, 512]), Buf(f"psV{i}")) for i in range(2)])
    t1s = Rot([(p.sb(f"t1_{i}", [128, 512], F32), Buf(f"t1_{i}")) for i in range(2)])
    t2s = Rot([(p.sb(f"t2_{i}", [128, 512], F32), Buf(f"t2_{i}")) for i in range(2)])
    obs = Rot([(p.sb(f"ob{i}", [128, 512], BF16), Buf(f"ob{i}")) for i in range(4)])
    ovs = Rot([(p.sb(f"ov{i}", [128, 390], BF16), Buf(f"ov{i}")) for i in range(2)])
    ogs = Rot([(p.sb(f"og{i}", [128, 2072], F32), Buf(f"og{i}")) for i in range(1)])
    for ov, b_ov in ovs.items:
        p.v("pool", "memset", ov[:], 1.0, W=[b_ov])

    for gi in range(ngroups):
        g0 = gi * 512
        for tl in range(4):
            r0 = g0 + tl * 128
            xt, b_xt = xts.next()
            xn, b_xn = xns.next()
            st, b_st = sts.next()
            p.dma(xt[:], x[r0:r0 + 128, :], b_xt.name, W=[b_xt])
            p.act(xn[:], xt[:], AF.Square, R=[b_xt], W=[b_xn, b_st], accum_out=st[:, 0:1])
            p.act(st[:, 1:2], st[:, 0:1], AF.Sqrt, R=[b_st], W=[b_st], scale=1.0 / D, bias=EPS)
            p.v("dve", "reciprocal", st[:, 2:3], st[:, 1:2], R=[b_st], W=[b_st])
            p.v("dve", "tensor_scalar", xn[:], xt[:], st[:, 2:3], None, ALU.mult, R=[b_xt, b_st], W=[b_xn])
            for k in range(8):
                p.tr(pst[:, k, :], xn[:, k * 128:(k + 1) * 128], ident[:], R=[b_xn, b_ident], W=[b_pst])
            p.v("dve", "tensor_tensor", hT[:, :, tl * 128:(tl + 1) * 128], pst[:, :, :],
                bc(gain_sb[:, :], [128, 8, 128], 2), ALU.mult, R=[b_pst, b_gain], W=[b_hT])
        for (oi, c0, rope, rawi) in ((A_TCH if tch is None else tch) if lvl >= 2 else []):
            pu, b_pu = psU.next()
            for k in range(8):
                p.mm(pu[:], wb[:, k, c0:c0 + 128], hT[:, k, :], start=(k == 0), stop=(k == 7), R=[b_wb, b_hT], W=[b_pu])
            if rope:
                rbase = max(c for c in rotmap if c <= c0)
                rc0 = rotmap[rbase] + (c0 - rbase)
                pr, b_pr = psR.next()
                for k in range(8):
                    p.mm(pr[:], wr[:, k, rc0:rc0 + 128], hT[:, k, :], start=(k == 0), stop=(k == 7), R=[b_wr, b_hT], W=[b_pr])
                t1, b_t1 = t1s.next()
                t2, b_t2 = t2s.next()
                p.v("dve", "tensor_tensor", t1[:], pu[:], cosT[:, g0:g0 + 512], ALU.mult, R=[b_pu, b_cos], W=[b_t1])
                p.v("dve", "tensor_tensor", t2[:], pr[:], sinT[:, g0:g0 + 512], ALU.mult, R=[b_pr, b_sin], W=[b_t2])
                ob, b_ob = obs.next()
                p.v(addeng, "tensor_tensor", ob[:], t1[:], t2[:], ALU.add, R=[b_t1, b_t2], W=[b_ob])
                p.dma(OT[oi, :, g0:g0 + 512], ob[:], b_ob.name, R=[b_ob], eng=oeng, is_out=True)
                if rawi is not None:
                    ob, b_ob = obs.next()
                    p.v("dve", "tensor_copy", ob[:], pu[:], R=[b_pu], W=[b_ob])
                    p.dma(OT[rawi, :, g0:g0 + 512], ob[:], b_ob.name, R=[b_ob], eng=oeng, is_out=True)
            else:
                ob, b_ob = obs.next()
                p.act(ob[:], pu[:], AF.Copy, R=[b_pu], W=[b_ob])
                p.dma(OT[oi, :, g0:g0 + 512], ob[:], b_ob.name, R=[b_ob], eng=oeng, is_out=True)
        for tl in range(4 if lvl >= 3 else 0):
            r0 = g0 + tl * 128
            ov, b_ov = ovs.next()
            og, b_og = ogs.next()
            ov4 = ov[:, :].rearrange("p (a g d) -> p a g d", a=3, g=2)
            for vi, (c0, n) in enumerate(A_VCH):
                pv, b_pv = psV.next()
                for k in range(8):
                    p.mm(pv[:, 0:n], hT[:, k, tl * 128:(tl + 1) * 128], wb[:, k, c0:c0 + n], start=(k == 0), stop=(k == 7),
                         R=[b_wb, b_hT], W=[b_pv])
                if vi < 3:
                    p.v("dve", "tensor_copy", ov4[:, vi, :, 0:64], pv[:, 0:128].rearrange("p (g d) -> p g d", g=2),
                        R=[b_pv], W=[b_ov])
                    if vi == 2:
                        p.act(og[:, 0:24], pv[:, 128:152], AF.Sigmoid, R=[b_pv], W=[b_og, b_pv])
                else:
                    o0 = 24 + (vi - 3) * 512
                    p.act(og[:, o0:o0 + 512], pv[:, 0:512], AF.Sigmoid, R=[b_pv], W=[b_og])
            p.dma(OV[r0:r0 + 128, :], ov[:], b_ov.name, R=[b_ov], eng=oeng, is_out=True)
            p.dma(OG[r0:r0 + 128, :], og[:], b_og.name, R=[b_og], eng=oeng, is_out=True)
    p.emit()
    p.close()
    return nc


NFC = 44


def load_cast(p, dst_fn, src_fn, nrow_chunks, ncols, stg, colw):
    ci = 0
    for k in range(nrow_chunks):
        for c0 in range(0, ncols, colw):
            c1 = min(ncols, c0 + colw)
            st, bst = stg.next()
            p.dma(st[:, 0:c1 - c0], src_fn(k, c0, c1), bst.name, W=[bst])
            eng = ["act", "dve", "pool"][ci % 3]
            ci += 1
            dst, bdst = dst_fn(k, c0, c1)
            if eng == "act":
                p.act(dst, st[:, 0:c1 - c0], AF.Copy, R=[bst], W=[bdst])
            else:
                p.v(eng, "tensor_copy", dst, st[:, 0:c1 - c0], R=[bst], W=[bdst])


def norm_T(p, xt, b_xt, npart, xn, b_xn, st, b_st, pst, b_pst, ident, b_ident, gain_sb, b_gain, hT_dst, b_hT):
    p.act(xn[0:npart, :], xt[0:npart, :], AF.Square, R=[b_xt], W=[b_xn, b_st], accum_out=st[0:npart, 0:1])
    p.act(st[0:npart, 1:2], st[0:npart, 0:1], AF.Sqrt, R=[b_st], W=[b_st], scale=1.0 / D, bias=EPS)
    p.v("dve", "reciprocal", st[0:npart, 2:3], st[0:npart, 1:2], R=[b_st], W=[b_st])
    p.v("dve", "tensor_scalar", xn[0:npart, :], xt[0:npart, :], st[0:npart, 2:3], None, ALU.mult, R=[b_xt, b_st], W=[b_xn])
    for k in range(8):
        p.tr(pst[:, k, 0:npart], xn[0:npart, k * 128:(k + 1) * 128], ident[0:npart, 0:npart], R=[b_xn, b_ident], W=[b_pst])
    p.v("dve", "tensor_tensor", hT_dst, pst[:, :, 0:npart], bc(gain_sb[:, :], [128, 8, npart], 2), ALU.mult,
        R=[b_pst, b_gain], W=[b_hT])


GS = 256


def build_C(ngroups=TOK // GS):
    nc = bass.Bass("TRN2", target_bir_lowering=False)
    xm = nc.dram_tensor("xm", [TOK, D], F32, kind="ExternalInput").ap()
    halo = nc.dram_tensor("halo", [2, D], F32, kind="ExternalInput").ap()
    w_up = nc.dram_tensor("w_up", [D, 2 * DFF], F32, kind="ExternalInput").ap()
    w_dn = nc.dram_tensor("w_dn", [DFF, D], F32, kind="ExternalInput").ap()
    cwd = nc.dram_tensor("cw", [128, NFC * 3], F32, kind="ExternalInput").ap()
    cbd = nc.dram_tensor("cb", [128, NFC], F32, kind="ExternalInput").ap()
    gain = nc.dram_tensor("gain", [128, 8], F32, kind="ExternalInput").ap()
    pgain = nc.dram_tensor("pgain", [1, D], F32, kind="ExternalInput").ap()
    identd = nc.dram_tensor("ident", [128, 128], F32, kind="ExternalInput").ap()
    xo = nc.dram_tensor("xo", [TOK, D], F32, kind="ExternalOutput").ap()
    p = Prog(nc)
    wub = p.sb("wub", [128, 8, 2 * DFF], BF16); b_wub = Buf("wub")
    wdb = p.sb("wdb", [128, 22, D], BF16); b_wdb = Buf("wdb")
    cw = p.sb("cw_sb", [128, NFC * 3], F32); b_cw = Buf("cw")
    cb = p.sb("cb_sb", [128, NFC], F32); b_cb = Buf("cb")
    gain_sb = p.sb("gain_sb", [128, 8], F32); b_gain = Buf("gain")
    pg = p.sb("pg", [128, D], F32); b_pg = Buf("pg")
    identf = p.sb("identf", [128, 128], F32); b_identf = Buf("identf")
    ident = p.sb("identb", [128, 128], BF16); b_ident = Buf("ident")
    p.dma(gain_sb[:], gain[:, :], "gain", W=[b_gain])
    p.dma(cw[:], cwd[:, :], "cw", W=[b_cw])
    p.dma(cb[:], cbd[:, :], "cb", W=[b_cb])
    p.dma(pg[:], pgain[0:1, :].broadcast_to([128, D]), "pg", W=[b_pg])
    p.dma(identf[:], identd[:, :], "identf", W=[b_identf])
    p.v("dve", "tensor_copy", ident[:], identf[:], R=[b_identf], W=[b_ident])
    SW = 512
    stg = Rot([(p.sb(f"stg{i}", [128, SW], F32), Buf(f"stg{i}")) for i in range(2)])
    load_cast(p, lambda k, c0, c1: (wub[:, k, c0:c1], b_wub), lambda k, c0, c1: w_up[k * 128:(k + 1) * 128, c0:c1], 8, 2 * DFF, stg, SW)
    load_cast(p, lambda k, c0, c1: (wdb[:, k, c0:c1], b_wdb), lambda k, c0, c1: w_dn[k * 128:(k + 1) * 128, c0:c1], 22, D, stg, SW)

    xts = [(p.sb(f"xt{i}", [128, D], F32), Buf(f"xt{i}")) for i in range(GS // 128)]
    xns = Rot([(p.sb(f"xn{i}", [128, D], BF16), Buf(f"xn{i}")) for i in range(2)])
    sts = Rot([(p.sb(f"st{i}", [128, 4], F32), Buf(f"stat{i}")) for i in range(2)])
    hT = p.sb("hT", [128, 8, GS], BF16); b_hT = Buf("hT")
    hTh = p.sb("hTh", [128, 8, 2], BF16); b_hTh = Buf("hTh")
    gT = p.sb("gT", [128, 22, GS], BF16); b_gT = Buf("gT")
    carry = p.sb("carry", [128, NFC, 2], F32)
    b_carry = [Buf(f"carry{c}") for c in range(NFC)]
    usbs = Rot([(p.sb(f"usb{i}", [128, GS + 2], F32), Buf(f"usb{i}")) for i in range(2)])
    accs = Rot([(p.sb(f"acc{i}", [128, GS], F32), Buf(f"acc{i}")) for i in range(4)])
    ys = Rot([(p.sb(f"y{i}", [128, D], F32), Buf(f"y{i}")) for i in range(2)])
    pst = p.ps("pst", [128, 8, 128], BF16); b_pst = Buf("pst")
    psu = Rot([(p.ps(f"psu{i}", [128, 512]), Buf(f"psu{i}")) for i in range(2)])
    psh = p.ps("psh", [128, 512]); b_psh = Buf("psh")
    psd = [(p.ps(f"psd{i}", [128, 512]), Buf(f"psd{i}")) for i in range(2)]

    xh, b_xh = xts[0]
    p.v("pool", "memset", xh[:], 0.0, W=[b_xh])
    p.dma(xh[0:2, :], halo[:, :], b_xh.name, R=[b_xh], W=[b_xh])
    xn, b_xn = xns.next()
    st, b_st = sts.next()
    norm_T(p, xh, b_xh, 128, xn, b_xn, st, b_st, pst, b_pst, ident, b_ident, gain_sb, b_gain, hT[:, :, 0:128], b_hT)
    p.v("dve", "tensor_copy", hTh[:, :, :], hT[:, :, 0:2], R=[b_hT], W=[b_hTh])
    for ch in range(NFC):
        for k in range(8):
            p.mm(psh[:, 2 * ch:2 * ch + 2], wub[:, k, ch * 128:(ch + 1) * 128], hTh[:, k, :], start=(k == 0), stop=(k == 7),
                 R=[b_wub, b_hTh], W=[b_psh])
    p.v("dve", "tensor_copy", carry[:, :, :], psh[:, 0:2 * NFC].rearrange("p (c t) -> p c t", t=2), R=[b_psh], W=b_carry)

    for gi in range(ngroups):
        g0 = gi * GS
        for tl in range(GS // 128):
            r0 = g0 + tl * 128
            xt, b_xt = xts[tl]
            xn, b_xn = xns.next()
            st, b_st = sts.next()
            p.dma(xt[:], xm[r0:r0 + 128, :], b_xt.name, W=[b_xt])
            norm_T(p, xt, b_xt, 128, xn, b_xn, st, b_st, pst, b_pst, ident, b_ident, gain_sb, b_gain,
                   hT[:, :, tl * 128:(tl + 1) * 128], b_hT)
        for c in range(22):
            accl = []
            for half, ch in enumerate((c, c + 22)):
                pu, b_pu = psu.next()
                for k in range(8):
                    p.mm(pu[:, 0:GS], wub[:, k, ch * 128:(ch + 1) * 128], hT[:, k, :], start=(k == 0), stop=(k == 7), R=[b_wub, b_hT], W=[b_pu])
                us, b_us = usbs.next()
                p.act(us[:, 2:GS + 2], pu[:, 0:GS], AF.Copy, R=[b_pu], W=[b_us])
                p.v("pool", "tensor_copy", us[:, 0:2], carry[:, ch, :], R=[b_carry[ch]], W=[b_us])
                p.v("pool", "tensor_copy", carry[:, ch, :], us[:, GS:GS + 2], R=[b_us], W=[b_carry[ch]])
                ac, b_ac = accs.next()
                eng = "dve"
                p.v("dve", "tensor_scalar", ac[:], us[:, 2:GS + 2], cw[:, 3 * ch + 2:3 * ch + 3], cb[:, ch:ch + 1], ALU.mult, ALU.add,
                    R=[b_us, b_cw, b_cb], W=[b_ac])
                p.v(eng, "scalar_tensor_tensor", ac[:], us[:, 1:GS + 1], cw[:, 3 * ch + 1:3 * ch + 2], ac[:], ALU.mult, ALU.add,
                    R=[b_us, b_cw, b_ac], W=[b_ac])
                p.v(eng, "scalar_tensor_tensor", ac[:], us[:, 0:GS], cw[:, 3 * ch:3 * ch + 1], ac[:], ALU.mult, ALU.add,
                    R=[b_us, b_cw, b_ac], W=[b_ac])
                accl.append((ac, b_ac))
            (aa, b_aa), (av, b_av) = accl
            p.act(aa[:], aa[:], AF.Gelu_apprx_tanh, R=[b_aa], W=[b_aa])
            p.v("dve", "tensor_tensor", gT[:, c, :], aa[:], av[:], ALU.mult, R=[b_aa, b_av], W=[b_gT])
        for tl in range(GS // 128):
            r0 = g0 + tl * 128
            xt, b_xt = xts[tl]
            st, b_st = sts.next()
            y, b_y = ys.next()
            for hh in range(2):
                pd, b_pd = psd[hh]
                for c in range(22):
                    p.mm(pd[:], gT[:, c, tl * 128:(tl + 1) * 128], wdb[:, c, hh * 512:(hh + 1) * 512], start=(c == 0), stop=(c == 21),
                         R=[b_gT, b_wdb], W=[b_pd])
                p.act(y[:, hh * 512:(hh + 1) * 512], pd[:], AF.Square, R=[b_pd], W=[b_y, b_st], accum_out=st[:, hh:hh + 1])
            p.v("dve", "tensor_tensor", st[:, 2:3], st[:, 0:1], st[:, 1:2], ALU.add, R=[b_st], W=[b_st])
            p.act(st[:, 3:4], st[:, 2:3], AF.Sqrt, R=[b_st], W=[b_st], scale=1.0 / D, bias=EPS)
            p.v("dve", "reciprocal", st[:, 2:3], st[:, 3:4], R=[b_st], W=[b_st])
            for hh in range(2):
                pd, b_pd = psd[hh]
                p.v("dve", "scalar_tensor_tensor", y[:, hh * 512:(hh + 1) * 512], pd[:], st[:, 2:3], pg[:, hh * 512:(hh + 1) * 512],
                    ALU.mult, ALU.mult, R=[b_pd, b_st, b_pg, b_y], W=[b_y])
            p.v("pool", "tensor_tensor", y[:], y[:], xt[:], ALU.add, R=[b_y, b_xt], W=[b_y])
            p.dma(xo[r0:r0 + 128, :], y[:], b_y.name, R=[b_y], eng="pool", is_out=True)
    p.emit()
    p.close()
    return nc


def run_C(nc, xm_cores, halo_cores, w_up, w_dn, conv_w, conv_b, pre_gain, post_gain):
    cst = const_inputs()
    cw = np.ascontiguousarray(conv_w.T.reshape(NFC, 128, 3).transpose(1, 0, 2).reshape(128, NFC * 3)).astype(np.float32)
    cbv = np.ascontiguousarray(conv_b.reshape(NFC, 128).T).astype(np.float32)
    maps = []
    for c in range(NCORES):
        maps.append({"xm": xm_cores[c], "halo": halo_cores[c], "w_up": w_up, "w_dn": w_dn, "cw": cw, "cb": cbv,
                     "gain": gain_layout(pre_gain), "pgain": post_gain.reshape(1, D).astype(np.float32), "ident": cst["ident"]})
    res = run_bass_kernel_spmd(nc, maps, core_ids=list(range(NCORES)))
    return res.results


NEG = -30000.0


def build_B(nblocks=NB):
    nc = bass.Bass("TRN2", target_bir_lowering=False)
    dI = lambda n, s, t: nc.dram_tensor(n, s, t, kind="ExternalInput").ap()
    QQd = dI("QQ", [NB, 128, 1536], BF16)
    KCd = dI("KC", [NB, 128, 1024], BF16)
    VCd = dI("VC", [NB, 128, 1040], BF16)
    KSFd = dI("KSF", [128, SEQ], BF16)
    VSFd = dI("VSF", [128, 128 * 130], BF16)
    KCBd = dI("KCBF", [128, SEQ + 16], BF16)
    VCBd = dI("VCBF", [128, SEQ + 16], BF16)
    OGd = dI("OGB", [NB, 128, 2072], F32)
    XBd = dI("XB", [NB, 128, D], F32)
    TQCd = dI("TQC", [128, 96], F32)
    TQRd = dI("TQR", [NB, 128], F32)
    CSTd = dI("CST", [128, 1552], F32)
    MSKd = dI("MSK", [128, 256], F32)
    EXd = dI("EX", [128, 8192], BF16)
    W1d = dI("W1", [2, 128, 4096], F32)
    POSTd = dI("POST", [64, 64], F32)
    B1d = dI("B1", [128, 2], F32)
    W2d = dI("W2", [128, 128], F32)
    WAd = dI("WA", [512, D], F32)
    WBd = dI("WB", [512, D], F32)
    WOd = dI("WO", [D, D], F32)
    PGd = dI("PG", [1, D], F32)
    SINKd = dI("SINK", [1, 8], F32)
    identd = dI("ident", [128, 128], F32)
    XMd = nc.dram_tensor("XM", [NB, 128, D], F32, kind="ExternalOutput").ap()
    p = Prog(nc)
    T = lambda n, s, t: (p.sb(n, s, t), Buf(n))

    big, b_big = T("big", [128, SEQ + 16], BF16)
    vsf, b_vsf = T("vsf", [128, 128 * 130], BF16)
    ex, b_ex = T("ex", [128, 8192], BF16)
    wab, b_wab = T("wab", [128, 8, D], BF16)
    wob, b_wob = T("wob", [128, 8, D], BF16)
    kcmpT, b_kcmpT = T("kcmpT", [128, 1024], BF16)
    vcmp, b_vcmp = T("vcmp", [128, 8, 130], BF16)
    cst, b_cst = T("cst", [128, 1552], F32)
    tqc, b_tqc = T("tqc", [128, 96], F32)
    mskf, b_mskf = T("mskf", [128, 256], F32)
    msk, b_msk = T("msk", [128, 256], BF16)
    pg, b_pg = T("pgb", [128, D], F32)
    esink, b_esink = T("esink", [128, 8], F32)
    identf, b_identf = T("identf", [128, 128], F32)
    ident, b_ident = T("identb", [128, 128], BF16)
    post, b_post = T("post", [64, 64], F32)
    postb, b_postb = T("postb", [64, 64], BF16)
    b1, b_b1 = T("b1", [128, 2], F32)
    beff, b_beff = T("beff", [128, 2], F32)
    w2f, b_w2f = T("w2f", [128, 128], F32)
    w2b, b_w2b = T("w2b", [128, 128], BF16)
    w2pad, b_w2pad = T("w2pad", [128, 2, 128], BF16)
    hid, b_hid = T("hid", [128, 2, 512], BF16)
    cmpend_row = cst[:, 0:1024]
    blk64 = cst[:, 1024:1280]
    b0row = cst[:, 1280:1536]
    cmpend_col = cst[:, 1536:1544]

    p.dma(cst[:], CSTd[:, :], "cst", W=[b_cst])
    p.dma(tqc[:], TQCd[:, :], "tqc", W=[b_tqc])
    p.dma(mskf[:], MSKd[:, :], "mskf", W=[b_mskf])
    p.v("dve", "tensor_copy", msk[:], mskf[:], R=[b_mskf], W=[b_msk])
    p.dma(pg[:], PGd[0:1, :].broadcast_to([128, D]), "pg", W=[b_pg])
    p.dma(esink[:], SINKd[0:1, :].broadcast_to([128, 8]), "esink", W=[b_esink])
    p.act(esink[:], esink[:], AF.Exp, R=[b_esink], W=[b_esink])
    p.dma(identf[:], identd[:, :], "identf", W=[b_identf])
    p.v("dve", "tensor_copy", ident[:], identf[:], R=[b_identf], W=[b_ident])
    p.dma(post[:], POSTd[:, :], "post", W=[b_post])
    p.v("dve", "tensor_copy", postb[:], post[:], R=[b_post], W=[b_postb])
    p.dma(b1[:], B1d[:, :], "b1", W=[b_b1])
    p.dma(w2f[:], W2d[:, :], "w2f", W=[b_w2f])
    p.v("dve", "tensor_copy", w2b[:], w2f[:], R=[b_w2f], W=[b_w2b])
    p.v("pool", "memset", w2pad[:], 0.0, W=[b_w2pad])
    p.v("dve", "tensor_copy", w2pad[:, 0, 0:64], w2f[:, 0:64], R=[b_w2f, b_w2pad], W=[b_w2pad])
    p.v("dve", "tensor_copy", w2pad[:, 1, 64:128], w2f[:, 0:64], R=[b_w2f, b_w2pad], W=[b_w2pad])
    p.dma(ex[:], EXd[:, :], "ex", W=[b_ex])

    SW = 1024
    stg = Rot([(p.sb(f"stg{i}", [128, SW], F32), Buf(f"stg{i}")) for i in range(2)])
    load_cast(p, lambda k, c0, c1: (vsf[:, k * 4096 + c0:k * 4096 + c1], b_vsf), lambda k, c0, c1: W1d[k, :, c0:c1], 2, 4096, stg, SW)
    load_cast(p, lambda k, c0, c1: (wab[:, k, c0:c1], b_wab), lambda k, c0, c1: WAd[k * 128:(k + 1) * 128, c0:c1], 4, D, stg, SW)
    load_cast(p, lambda k, c0, c1: (wab[:, 4 + k, c0:c1], b_wab), lambda k, c0, c1: WBd[k * 128:(k + 1) * 128, c0:c1], 4, D, stg, SW)
    load_cast(p, lambda k, c0, c1: (wob[:, k, c0:c1], b_wob), lambda k, c0, c1: WOd[k * 128:(k + 1) * 128, c0:c1], 8, D, stg, SW)

    ps_s = Rot([(p.ps(f"ps_s{i}", [128, 4, 128]), Buf(f"ps_s{i}")) for i in range(2)])
    ps_o = [(p.ps(f"ps_o{i}", [128, 4, 128]), Buf(f"ps_o{i}")) for i in range(2)]
    ps_c, b_ps_c = p.ps("ps_c", [128, 1024]), Buf("ps_c")
    ps_t, b_ps_t = p.ps("ps_t", [128, 8, 128], BF16), Buf("ps_t")
    ps_y, b_ps_y = p.ps("ps_y", [128, 512]), Buf("ps_y")

    w1v = vsf[:, 0:8192].rearrange("p (k j h) -> p k j h", k=2, j=32)
    for kv in range(2):
        p.dma(big[:], (KCBd if kv == 0 else VCBd)[:, :], "big", W=[b_big])
        for j in range(32):
            p.mm(ps_y[:, 0:1], w1v[0:64, kv, j, :], postb[:, kv * 32 + j:kv * 32 + j + 1], start=(j == 0), stop=(j == 31),
                 R=[b_vsf, b_postb], W=[b_ps_y])
        p.v("dve", "tensor_tensor", beff[:, kv:kv + 1], ps_y[:, 0:1], b1[:, kv:kv + 1], ALU.add, R=[b_ps_y, b_b1], W=[b_beff])
        for hf in range(2):
            c0 = hf * 512
            for g in range(2):
                rows = slice(g * 64, (g + 1) * 64)
                for j in range(32):
                    p.mm(ps_c[:, 0:512], w1v[rows, kv, j, :], big[rows, 16 * c0 + j:16 * c0 + j + 16 * 511 + 1:16],
                         start=(j == 0), stop=(j == 31), R=[b_vsf, b_big], W=[b_ps_c])
                p.act(hid[:, g, :], ps_c[:, 0:512], AF.Gelu_apprx_tanh, R=[b_ps_c, b_beff], W=[b_hid], bias=beff[:, kv:kv + 1])
            if kv == 0:
                for g in range(2):
                    p.mm(ps_y[:, :], w2pad[:, g, :], hid[:, g, :], start=(g == 0), stop=(g == 1), R=[b_w2pad, b_hid], W=[b_ps_y])
                p.v("dve", "tensor_copy", kcmpT[:, c0:c0 + 512], ps_y[:, :], R=[b_ps_y], W=[b_kcmpT])
            else:
                for ct in range(4):
                    for g in range(2):
                        p.mm(ps_y[:, g * 64:(g + 1) * 64], hid[:, g, ct * 128:(ct + 1) * 128], w2b[:, 64:128], start=True, stop=True,
                             R=[b_hid, b_w2b], W=[b_ps_y])
                    p.v("dve", "tensor_copy", vcmp[:, hf * 4 + ct, :].rearrange("p (g e) -> p g e", g=2)[:, :, 0:64],
                        ps_y[:, 0:128].rearrange("p (g d) -> p g d", g=2), R=[b_ps_y, b_vcmp], W=[b_vcmp])
    vc4 = vcmp[:, :, :].rearrange("p k (g e) -> p k g e", g=2)
    p.v("pool", "memset", vc4[:, :, :, 64:65], 1.0, R=[b_vcmp], W=[b_vcmp])
    p.dma(big[:, 0:SEQ], KSFd[:, :], "big", W=[b_big])
    p.dma(vsf[:], VSFd[:, :], "vsf", W=[b_vsf])

    qq, b_qq = T("qq", [128, 3, 4, 128], BF16)
    kc, b_kc = T("kc", [128, 8, 128], BF16)
    vc, b_vc = T("vc", [128, 8, 130], BF16)
    ogb, b_ogb = T("ogb", [128, 2072], F32)
    xb, b_xb = T("xb", [128, D], F32)
    tqr, b_tqr = T("tqr", [128, 128], F32)
    ec, b_ec = T("ec", [128, 1024], F32)
    mkc, b_mkc = T("mkc", [128, 1024], F32)
    imp, b_imp = T("imp", [128, 1024], F32)
    mct, b_mct = T("mct", [128, 128], BF16)
    sv = {n: T("sv_" + n, [128, 256], F32) for n in ("valid", "bonus", "vm1", "notown", "imps", "v", "v2", "sel")}
    biasb, b_biasb = T("biasb", [128, 256], BF16)
    biasT4, b_biasT4 = T("biasT4", [128, 2, 4, 128], BF16)
    pTs = Rot([T(f"pT{i}", [128, 4, 128], BF16) for i in range(4)])
    st, b_st = T("stt", [128, 64], F32)
    oa, b_oa = T("oa", [128, 8, 64], BF16)
    ob32, b_ob32 = T("ob32", [128, 8, 64], F32)
    otmp, b_otmp = T("otmp", [128, 8, 64], F32)
    obb, b_obb = T("obb", [128, 8, 64], BF16)
    oT, b_oT = T("oT", [128, 8, 128], BF16)
    y32, b_y32 = T("y32", [128, D], F32)
    mixb, b_mixb = T("mixb", [128, D], BF16)
    p.v("pool", "memset", biasb[:], 0.0, W=[b_biasb])
    acc = p.sb("acc", [128, 2, 4, 65], F32)
    b_acc = [Buf("acc0"), Buf("acc1")]

    def accum(g, first):
        po, b_po = ps_o[g]
        if first:
            p.v("dve", "tensor_copy", acc[:, g, :, :], po[:, :, 0:65], R=[b_po], W=[b_acc[g]])
        else:
            p.v("dve", "tensor_tensor", acc[:, g, :, :], acc[:, g, :, :], po[:, :, 0:65], ALU.add, R=[b_po, b_acc[g]], W=[b_acc[g]])

    def branch(tiles, qm, po_list):
        nt = len(tiles)
        for ti, (kfn, vfn, mask, bias) in enumerate(tiles):
            for g in range(2):
                rows = slice(g * 64, (g + 1) * 64)
                ps, b_ps = ps_s.next()
                kap, kbufs = kfn(g)
                p.mm(ps[:, :, :], kap, qq[rows, qm, :, :], start=True, stop=(bias is None), R=kbufs + [b_qq], W=[b_ps])
                if bias is not None:
                    lap, rap = bias
                    p.mm(ps[:, :, :], lap, rap, start=False, stop=True, R=[b_ex, b_biasT4], W=[b_ps])
                pT, b_pT = pTs.next()
                p.act(pT[:, :, :], ps[:, :, :], AF.Exp, R=[b_ps], W=[b_pT], scale=0.125)
                if mask is not None:
                    map_, mbufs = mask
                    p.v("pool", "tensor_tensor", pT[:, :, :], pT[:, :, :], bc(map_, [128, 4, 128], 1), ALU.mult, R=[b_pT] + mbufs, W=[b_pT])
                vap, vbufs = vfn(g)
                po, b_po = po_list[g]
                for hh in range(4):
                    p.mm(po[:, hh, 0:65], pT[:, hh, :], vap, start=True, stop=True, R=[b_pT] + vbufs, W=[b_po])
                accum(g, ti == 0)

    def normalize(dst_ap, dst_bufs, gate_col0, extra_den, first, eng="dve"):
        for g in range(2):
            po, b_po = ps_o[g]
            den = st[:, 8 + 4 * g:12 + 4 * g]
            p.v("dve", "tensor_copy", den, acc[:, g, :, 64], R=[b_acc[g], b_st], W=[b_st])
            if extra_den is not None:
                p.v("dve", "tensor_tensor", den, den, extra_den[:, 4 * g:4 * g + 4], ALU.add, R=[b_st, b_esink], W=[b_st])
            p.v("dve", "tensor_scalar", den, den, 1e-30, None, ALU.max, R=[b_st], W=[b_st])
            p.v("dve", "reciprocal", den, den, R=[b_st], W=[b_st])
            if gate_col0 is not None:
                gv = ogb[:, 12 * g:12 * g + 12].rearrange("p (h t) -> p h t", t=3)[:, :, gate_col0]
                p.v("dve", "tensor_tensor", den, den, gv, ALU.mult, R=[b_st, b_ogb], W=[b_st])
            tgt = dst_ap[:, 4 * g:4 * g + 4, :] if first else otmp[:, 4 * g:4 * g + 4, :]
            p.v("dve", "tensor_tensor", tgt, acc[:, g, :, 0:64], bc(den, [128, 4, 64], 2), ALU.mult,
                R=[b_acc[g], b_st], W=(dst_bufs if first else [b_otmp]))
            if not first:
                p.v("pool", "tensor_tensor", dst_ap[:, 4 * g:4 * g + 4, :], dst_ap[:, 4 * g:4 * g + 4, :], otmp[:, 4 * g:4 * g + 4, :], ALU.add,
                    R=dst_bufs + [b_otmp], W=dst_bufs)

    kcf = lambda t: (lambda g: (kc[g * 64:(g + 1) * 64, t, :], [b_kc]))
    vcf = lambda t: (lambda g: (vc[:, t, g * 65:(g + 1) * 65], [b_vc]))
    mD = (msk[:, 0:128], [b_msk])
    mP = (msk[:, 128:256], [b_msk])

    for j in range(nblocks):
        p.dma(qq[:, :, :, :].rearrange("p a h q -> p (a h q)"), QQd[j], "qq", W=[b_qq])
        p.dma(kc[:, :, :].rearrange("p t k -> p (t k)"), KCd[j], "kc", W=[b_kc])
        p.dma(vc[:, :, :].rearrange("p t k -> p (t k)"), VCd[j], "vc", W=[b_vc])
        p.dma(ogb[:], OGd[j], "ogb", W=[b_ogb])
        p.dma(xb[:], XBd[j], "xb", W=[b_xb])
        p.dma(tqr[:], TQRd[j:j + 1, :].broadcast_to([128, 128]), "tqr", W=[b_tqr])
        tq_col = tqc[:, j:j + 1]
        tm128_col = tqc[:, 32 + j:33 + j]
        t0_col = tqc[:, 64 + j:65 + j]
        branch([(kcf(0), vcf(0), mP, None), (kcf(1), vcf(1), mD, None)], 0, ps_o)
        normalize(oa, [b_oa], None, esink, True)
        branch([(kcf(2), vcf(2), mP, None), (kcf(3), vcf(3), None, None), (kcf(4), vcf(4), None, None),
                (kcf(5), vcf(5), None, None), (kcf(6), vcf(6), mD, None)], 1, ps_o)
        normalize(ob32, [b_ob32], 2, None, True)
        nkt = (4 * j + 3) // 16 + 1
        ncols = 128 * nkt
        nblk = 8 * j + 8
        p.v("dve", "tensor_scalar", mkc[:, 0:ncols], cmpend_row[:, 0:ncols], tq_col, None, ALU.is_le, R=[b_cst, b_tqc], W=[b_mkc])
        va, b_va = sv["valid"]; bo, b_bo = sv["bonus"]; vm1, b_vm1 = sv["vm1"]; no, b_no = sv["notown"]
        p.v("dve", "tensor_scalar", va[:, 0:nblk], blk64[:, 0:nblk], tq_col, None, ALU.is_le, R=[b_cst, b_tqc], W=[b_va])
        p.v("dve", "tensor_scalar", bo[:, 0:nblk], blk64[:, 0:nblk], tm128_col, 100.0, ALU.is_gt, ALU.mult, R=[b_cst, b_tqc], W=[b_bo])
        p.v("dve", "tensor_tensor", bo[:, 0:nblk], bo[:, 0:nblk], b0row[:, 0:nblk], ALU.add, R=[b_bo, b_cst], W=[b_bo])
        p.v("dve", "tensor_scalar", vm1[:, 0:nblk], va[:, 0:nblk], -1.0, None, ALU.add, R=[b_va], W=[b_vm1])
        p.v("dve", "tensor_scalar", no[:, 0:nblk], blk64[:, 0:nblk], t0_col, None, ALU.is_lt, R=[b_cst, b_tqc], W=[b_no])
        sel_bias = []
        for g in range(2):
            rows = slice(g * 64, (g + 1) * 64)
            for hh in range(4):
                for c0 in range(0, ncols, 512):
                    c1 = min(ncols, c0 + 512)
                    p.mm(ps_c[:, c0:c1], qq[rows, 2, hh, :], kcmpT[rows, c0:c1], start=True, stop=True, R=[b_qq, b_kcmpT], W=[b_ps_c])
                p.act(ec[:, 0:ncols], ps_c[:, 0:ncols], AF.Exp, R=[b_ps_c], W=[b_ec], scale=0.125)
                dcol = st[:, 16 + hh:17 + hh]
                p.v("dve", "scalar_tensor_tensor", ec[:, 0:ncols], ec[:, 0:ncols], 1.0, mkc[:, 0:ncols], ALU.mult, ALU.mult,
                    R=[b_ec, b_mkc, b_st], W=[b_ec, b_st], accum_out=dcol)
                p.v("dve", "tensor_scalar", dcol, dcol, 1e-30, None, ALU.max, R=[b_st], W=[b_st])
                p.v("dve", "reciprocal", dcol, dcol, R=[b_st], W=[b_st])
                if hh == 0:
                    p.v("dve", "tensor_scalar", imp[:, 0:ncols], ec[:, 0:ncols], dcol, None, ALU.mult, R=[b_ec, b_st], W=[b_imp])
                else:
                    p.v("dve", "scalar_tensor_tensor", imp[:, 0:ncols], ec[:, 0:ncols], dcol, imp[:, 0:ncols], ALU.mult, ALU.add,
                        R=[b_ec, b_st, b_imp], W=[b_imp])
            iv = imp[:, 0:4 * nblk].rearrange("p (b f) -> p b f", f=4)
            ims, b_ims = sv["imps"]; vv, b_vv = sv["v"]; v2, b_v2 = sv["v2"]; se, b_se = sv["sel"]
            p.v("dve", "tensor_tensor", ims[:, 0:nblk], iv[:, :, 0], iv[:, :, 1], ALU.add, R=[b_imp], W=[b_ims])
            p.v("dve", "tensor_tensor", ims[:, 0:nblk], ims[:, 0:nblk], iv[:, :, 2], ALU.add, R=[b_imp, b_ims], W=[b_ims])
            p.v("dve", "scalar_tensor_tensor", ims[:, 0:nblk], ims[:, 0:nblk], 2.0, iv[:, :, 3], ALU.mult, ALU.add, R=[b_imp, b_ims], W=[b_ims])
            p.v("dve", "tensor_tensor", vv[:, 0:nblk], ims[:, 0:nblk], bo[:, 0:nblk], ALU.add, R=[b_ims, b_bo], W=[b_vv])
            p.v("dve", "tensor_tensor", vv[:, 1:nblk], vv[:, 1:nblk], iv[:, 0:nblk - 1, 3], ALU.add, R=[b_imp, b_vv], W=[b_vv])
            p.v("dve", "tensor_tensor", vv[:, 0:nblk], vv[:, 0:nblk], va[:, 0:nblk], ALU.mult, R=[b_vv, b_va], W=[b_vv])
            p.v("dve", "tensor_tensor", vv[:, 0:nblk], vv[:, 0:nblk], vm1[:, 0:nblk], ALU.add, R=[b_vv, b_vm1], W=[b_vv])
            p.v("dve", "max", st[:, 24:32], vv[:, 0:nblk], R=[b_vv, b_st], W=[b_st])
            p.v("dve", "match_replace", v2[:, 0:nblk], st[:, 24:32], vv[:, 0:nblk], -2.0, R=[b_vv, b_st], W=[b_v2])
            p.v("dve", "max", st[:, 32:40], v2[:, 0:nblk], R=[b_v2, b_st], W=[b_st])
            p.v("dve", "tensor_scalar", se[:, 0:nblk], vv[:, 0:nblk], st[:, 39:40], None, ALU.is_ge, R=[b_vv, b_st], W=[b_se])
            p.v("dve", "tensor_tensor", se[:, 0:nblk], se[:, 0:nblk], va[:, 0:nblk], ALU.mult, R=[b_se, b_va], W=[b_se])
            p.v("dve", "tensor_tensor", se[:, 0:nblk], se[:, 0:nblk], no[:, 0:nblk], ALU.mult, R=[b_se, b_no], W=[b_se])
            p.v("dve", "tensor_scalar", biasb[:, 0:nblk], se[:, 0:nblk], -1.0, -NEG, ALU.add, ALU.mult, R=[b_se, b_biasb], W=[b_biasb])
            nch = (nblk + 127) // 128
            for ch in range(nch):
                p.tr(ps_t[:, ch, :], biasb[:, ch * 128:(ch + 1) * 128], ident[:, :], R=[b_biasb, b_ident], W=[b_ps_t])
            sel_bias.append(nch)
            for ch in range(nch):
                p.v("dve", "tensor_copy", biasT4[:, ch, :, :], bc(ps_t[:, ch, :], [128, 4, 128], 1), R=[b_ps_t, b_biasT4], W=[b_biasT4])
            po, b_po = ps_o[g]
            ntile = 4 * j + 3
            for kt in range(ntile + 1):
                diag = (kt == ntile)
                ps, b_ps = ps_s.next()
                if diag:
                    p.mm(ps[:, :, :], kc[rows, 7, :], qq[rows, 1, :, :], start=True, stop=True, R=[b_kc, b_qq], W=[b_ps])
                else:
                    p.mm(ps[:, :, :], big[rows, kt * 128:(kt + 1) * 128], qq[rows, 1, :, :], start=True, stop=False, R=[b_big, b_qq], W=[b_ps])
                    p.mm(ps[:, :, :], ex[:, (kt % 64) * 128:(kt % 64 + 1) * 128], biasT4[:, kt // 64, :, :], start=False, stop=True,
                         R=[b_ex, b_biasT4], W=[b_ps])
                pT, b_pT = pTs.next()
                p.act(pT[:, :, :], ps[:, :, :], AF.Exp, R=[b_ps], W=[b_pT], scale=0.125)
                if diag:
                    p.v("pool", "tensor_tensor", pT[:, :, :], pT[:, :, :], bc(msk[:, 0:128], [128, 4, 128], 1), ALU.mult, R=[b_pT, b_msk], W=[b_pT])
                    vap = vc[:, 7, g * 65:(g + 1) * 65]
                    vb = b_vc
                else:
                    vap = vsf[:, kt * 130 + g * 65:kt * 130 + (g + 1) * 65]
                    vb = b_vsf
                for hh in range(4):
                    p.mm(po[:, hh, 0:65], pT[:, hh, :], vap, start=True, stop=True, R=[b_pT, vb], W=[b_po])
                accum(g, kt == 0)
        normalize(ob32, [b_ob32], 1, None, False)
        tiles = []
        for kt in range(nkt):
            p.v("dve", "tensor_scalar", mct[:, :], tqr[:, :], cmpend_col[:, kt:kt + 1], None, ALU.is_ge, R=[b_tqr, b_cst, b_mct], W=[b_mct])
            branch([((lambda g, kt=kt: (kcmpT[g * 64:(g + 1) * 64, kt * 128:(kt + 1) * 128], [b_kcmpT])),
                     (lambda g, kt=kt: (vcmp[:, kt, g * 65:(g + 1) * 65], [b_vcmp])), (mct[:, :], [b_mct]), None)], 2, ps_o) if False else None
            for g in range(2):
                rows = slice(g * 64, (g + 1) * 64)
                ps, b_ps = ps_s.next()
                p.mm(ps[:, :, :], kcmpT[rows, kt * 128:(kt + 1) * 128], qq[rows, 2, :, :], start=True, stop=True, R=[b_kcmpT, b_qq], W=[b_ps])
                pT, b_pT = pTs.next()
                p.act(pT[:, :, :], ps[:, :, :], AF.Exp, R=[b_ps], W=[b_pT], scale=0.125)
                p.v("pool", "tensor_tensor", pT[:, :, :], pT[:, :, :], bc(mct[:, :], [128, 4, 128], 1), ALU.mult, R=[b_pT, b_mct], W=[b_pT])
                po, b_po = ps_o[g]
                for hh in range(4):
                    p.mm(po[:, hh, 0:65], pT[:, hh, :], vcmp[:, kt, g * 65:(g + 1) * 65], start=True, stop=True,
                         R=[b_pT, b_vcmp], W=[b_po])
                accum(g, kt == 0)
        normalize(ob32, [b_ob32], 0, None, False)
        p.v("dve", "tensor_copy", obb[:, :, :], ob32[:, :, :], R=[b_ob32], W=[b_obb])
        oaf = oa[:, :, :].rearrange("p h d -> p (h d)")
        obf = obb[:, :, :].rearrange("p h d -> p (h d)")
        for k in range(4):
            p.tr(ps_t[:, k, :], oaf[:, k * 128:(k + 1) * 128], ident[:, :], R=[b_oa, b_ident], W=[b_ps_t])
            p.tr(ps_t[:, 4 + k, :], obf[:, k * 128:(k + 1) * 128], ident[:, :], R=[b_obb, b_ident], W=[b_ps_t])
        p.v("dve", "tensor_copy", oT[:, :, :], ps_t[:, :, :], R=[b_ps_t], W=[b_oT])
        for hh in range(2):
            cs = slice(hh * 512, (hh + 1) * 512)
            for br in range(2):
                for k in range(4):
                    p.mm(ps_y[:, :], oT[:, 4 * br + k, :], wab[:, 4 * br + k, cs], start=(k == 0), stop=(k == 3), R=[b_oT, b_wab], W=[b_ps_y])
                gsl = slice(24 + br * 1024 + hh * 512, 24 + br * 1024 + (hh + 1) * 512)
                if br == 0:
                    p.v("dve", "tensor_tensor", y32[:, cs], ps_y[:, :], ogb[:, gsl], ALU.mult, R=[b_ps_y, b_ogb], W=[b_y32])
                else:
                    p.v("dve", "tensor_tensor", ec[:, 0:512], ps_y[:, :], ogb[:, gsl], ALU.mult, R=[b_ps_y, b_ogb], W=[b_ec])
                    p.v("pool", "tensor_tensor", mixb[:, cs], y32[:, cs], ec[:, 0:512], ALU.add, R=[b_y32, b_ec], W=[b_mixb])
        for k in range(8):
            p.tr(ps_t[:, k, :], mixb[:, k * 128:(k + 1) * 128], ident[:, :], R=[b_mixb, b_ident], W=[b_ps_t])
        p.v("dve", "tensor_copy", oT[:, :, :], ps_t[:, :, :], R=[b_ps_t], W=[b_oT])
        for hh in range(2):
            cs = slice(hh * 512, (hh + 1) * 512)
            for k in range(8):
                p.mm(ps_c[:, cs], oT[:, k, :], wob[:, k, cs], start=(k == 0), stop=(k == 7), R=[b_oT, b_wob], W=[b_ps_c])
        p.act(y32[:, :], ps_c[:, :], AF.Square, R=[b_ps_c], W=[b_y32, b_st], accum_out=st[:, 0:1])
        p.act(st[:, 1:2], st[:, 0:1], AF.Sqrt, R=[b_st], W=[b_st], scale=1.0 / D, bias=EPS)
        p.v("dve", "reciprocal", st[:, 2:3], st[:, 1:2], R=[b_st], W=[b_st])
        p.v("dve", "scalar_tensor_tensor", y32[:, :], ps_c[:, :], st[:, 2:3], pg[:, :], ALU.mult, ALU.mult, R=[b_ps_c, b_st, b_pg, b_y32], W=[b_y32])
        p.v("pool", "tensor_tensor", y32[:, :], y32[:, :], xb[:, :], ALU.add, R=[b_y32, b_xb], W=[b_y32])
        p.dma(XMd[j], y32[:, :], "y32", R=[b_y32], eng="pool", is_out=True)
    p.emit()
    p.close()
    return nc


def const_inputs():
    half = 32
    inv_freq = (10000.0 ** (-np.arange(half, dtype=np.float32) / half)).astype(np.float32)
    invf = np.tile(inv_freq, 4).reshape(128, 1).astype(np.float32)
    return {"invf": invf, "ident": np.eye(128, dtype=np.float32)}


def gain_layout(g):
    return np.ascontiguousarray(g.reshape(8, 128).T).astype(np.float32)


def run_A(nc, x_cores, pos_cores, w_in_l, gain_l):
    cst = const_inputs()
    maps = []
    for c in range(NCORES):
        maps.append({"x": x_cores[c], "pos": pos_cores[c], "w_in": w_in_l, "gain": gain_layout(gain_l),
                     "invf": cst["invf"], "ident": cst["ident"]})
    res = run_bass_kernel_spmd(nc, maps, core_ids=list(range(NCORES)))
    return res.results


def prep_B(resA, x, lw):
    bf = resA[0]["OT"].dtype
    cst = const_inputs()
    c_ = np.arange(1024, dtype=np.float32)
    CST = np.zeros((128, 1552), np.float32)
    CST[:, 0:1024] = 16.0 * c_ + 31.0
    CST[:, 1024:1280] = 64.0 * np.arange(256, dtype=np.float32)
    CST[:, 1280] = 100.0
    CST[:, 1536:1544] = 16.0 * (128.0 * np.arange(8)[None, :] + np.arange(128)[:, None]) + 31.0
    kk = np.arange(128)
    MSK = np.concatenate([(kk[:, None] <= kk[None, :]), (kk[:, None] > kk[None, :])], axis=1).astype(np.float32)
    EX = np.zeros((128, 64, 128), np.float32)
    for m in range(64):
        EX[2 * m, m, 0:64] = 1.0
        EX[2 * m + 1, m, 64:128] = 1.0
    EX = EX.reshape(128, 8192).astype(bf)
    w1 = lw["cmp_w1"]
    W1 = np.stack([np.tile(w1[kv].reshape(32, 64, 128).transpose(1, 0, 2).reshape(64, 4096), (2, 1)) for kv in range(2)]).astype(np.float32)
    POST = np.concatenate([lw["cmp_pos_emb"][kv].T for kv in range(2)], axis=1).astype(np.float32)
    B1 = np.ascontiguousarray(lw["cmp_b1"].T).astype(np.float32)
    W2 = np.concatenate([lw["cmp_w2"][0], lw["cmp_w2"][1]], axis=1).astype(np.float32)
    maps = []
    for b in range(2):
        OT = np.concatenate([resA[4 * b + r]["OT"] for r in range(4)], axis=2)
        OV = np.concatenate([resA[4 * b + r]["OV"] for r in range(4)], axis=0).reshape(SEQ, 3, 130)
        OG = np.concatenate([resA[4 * b + r]["OG"] for r in range(4)], axis=0)

        def qlay(base):
            return OT[base:base + 4].reshape(2, 4, 64, SEQ).transpose(0, 2, 1, 3).reshape(128, 4, SEQ)
        Q3 = np.stack([qlay(0), qlay(5), qlay(9)], axis=1)
        KA, KS, KW = OT[4], OT[15], OT[16]
        pad = np.zeros((128, 16), bf)
        KCBF = np.concatenate([OT[13], pad], axis=1)
        VCBF = np.concatenate([OT[14], pad], axis=1)
        VSF = np.ascontiguousarray(OV[:, 1].reshape(128, 128, 130).transpose(1, 0, 2)).reshape(128, 128 * 130)
        KSF = np.ascontiguousarray(KS)
        for r in range(4):
            QQ = np.zeros((NB, 128, 3, 4, 128), bf)
            KC = np.zeros((NB, 128, 8, 128), bf)
            VC = np.zeros((NB, 128, 8, 130), bf)
            OGB = np.zeros((NB, 128, 2072), np.float32)
            XB = np.zeros((NB, 128, D), np.float32)
            for j in range(NB):
                i = 4 * j + r
                tok = slice(i * 128, (i + 1) * 128)
                QQ[j] = Q3[:, :, :, tok]
                srcs = [(KA, 0, i - 1), (KA, 0, i)] + [(KW, 2, i - 4 + m) for m in range(5)] + [(KS, 1, i)]
                for t, (ksrc, vi, blk) in enumerate(srcs):
                    if blk >= 0:
                        KC[j, :, t, :] = ksrc[:, blk * 128:(blk + 1) * 128]
                        VC[j, :, t, :] = OV[blk * 128:(blk + 1) * 128, vi]
                OGB[j] = OG[tok]
                XB[j] = x[b, tok]
            tq = (128.0 * (4 * np.arange(NB)[None, :] + r) + np.arange(128)[:, None]).astype(np.float32)
            t0 = np.broadcast_to(128.0 * (4 * np.arange(NB)[None, :] + r), (128, NB)).astype(np.float32)
            TQC = np.concatenate([tq, tq - 128.0, t0], axis=1).astype(np.float32)
            maps.append({"QQ": QQ.reshape(NB, 128, 1536), "KC": KC.reshape(NB, 128, 1024), "VC": VC.reshape(NB, 128, 1040),
                         "KSF": KSF, "VSF": VSF, "KCBF": KCBF, "VCBF": VCBF, "OGB": OGB, "XB": XB, "TQC": TQC,
                         "TQR": np.ascontiguousarray(tq.T), "CST": CST, "MSK": MSK, "EX": EX, "W1": W1, "POST": POST, "B1": B1, "W2": W2,
                         "WA": lw["w_branch_a"], "WB": lw["w_branch_b"], "WO": lw["w_out"],
                         "PG": lw["attn_post_gain"].reshape(1, D).astype(np.float32), "SINK": lw["attn_sinks"].reshape(1, 8).astype(np.float32),
                         "ident": cst["ident"]})
    return maps


_PROGS = {}


def _prog(name, fn):
    if name not in _PROGS:
        _PROGS[name] = fn()
    return _PROGS[name]


def kernel(**inputs):
    inp = {k: np.asarray(v) for k, v in inputs.items()}
    x = np.ascontiguousarray(inp["x"], dtype=np.float32)
    pos = inp["positions"].astype(np.int32)
    pc = [np.ascontiguousarray(pos[c // 4, (c % 4) * TOK:(c % 4 + 1) * TOK]).reshape(1, TOK) for c in range(NCORES)]
    wnames = ["attn_pre_gain", "attn_post_gain", "ffn_pre_gain", "ffn_post_gain", "w_in", "attn_sinks", "cmp_pos_emb", "cmp_w1",
              "cmp_b1", "cmp_w2", "w_branch_a", "w_branch_b", "w_out", "ffn_w_up", "ffn_conv_w", "ffn_conv_b", "ffn_w_down"]
    for l in range(4):
        lw = {k: np.ascontiguousarray(inp[k][l], dtype=np.float32) for k in wnames}
        xc = [np.ascontiguousarray(x[c // 4, (c % 4) * TOK:(c % 4 + 1) * TOK]) for c in range(NCORES)]
        resA = run_A(_prog("A", build_A), xc, pc, lw["w_in"], lw["attn_pre_gain"])
        maps = prep_B(resA, x, lw)
        resB = run_bass_kernel_spmd(_prog("B", build_B), maps, core_ids=list(range(NCORES))).results
        xm = np.zeros_like(x)
        for c in range(NCORES):
            b, r = c // 4, c % 4
            xm[b].reshape(128, 128, D)[r::4] = resB[c]["XM"]
        xmc = [np.ascontiguousarray(xm[c // 4, (c % 4) * TOK:(c % 4 + 1) * TOK]) for c in range(NCORES)]
        hc = [np.ascontiguousarray(xm[c // 4, (c % 4) * TOK - 2:(c % 4) * TOK]) if c % 4 else np.zeros((2, D), np.float32)
              for c in range(NCORES)]
        resC = run_C(_prog("C", build_C), xmc, hc, lw["ffn_w_up"], lw["ffn_w_down"], lw["ffn_conv_w"], lw["ffn_conv_b"],
                     lw["ffn_pre_gain"], lw["ffn_post_gain"])
        x = np.stack([np.concatenate([resC[4 * b + r]["xo"] for r in range(4)], axis=0) for b in range(2)]).astype(np.float32)
    return x
```

```python
import numpy as np
from contextlib import ExitStack
import concourse.bass as bass
import concourse.mybir as mybir
from concourse.bass_utils import run_bass_kernel_spmd

F32 = mybir.dt.float32
BF16 = mybir.dt.bfloat16
I32 = mybir.dt.int32
ALU = mybir.AluOpType
AF = mybir.ActivationFunctionType

NCORES = 8
D = 1024
SEQ = 16384
TOK = 4096
NB = 32
INC = 4120
DFF = 2816
EPS = 1e-6


class Buf:
    __slots__ = ("name", "last_w", "readers")

    def __init__(self, name):
        self.name = name
        self.last_w = None
        self.readers = []


class Op:
    __slots__ = ("eng", "fn", "deps", "signal", "sig", "semkey", "dma", "idx")


class Prog:
    def __init__(self, nc):
        self.nc = nc
        self.ops = []
        self.stack = ExitStack()
        self.n_dma = {}
        self.out_dma_keys = set()

    def sb(self, name, shape, dt):
        return self.stack.enter_context(self.nc.sbuf_tensor(name, list(shape), dt))

    def ps(self, name, shape, dt=F32):
        return self.stack.enter_context(self.nc.psum_tensor(name, list(shape), dt))

    def add(self, eng, fn, R=(), W=(), dma=None):
        op = Op()
        op.eng = eng
        op.fn = fn
        op.signal = False
        op.sig = None
        op.dma = dma
        op.semkey = None
        op.idx = len(self.ops)
        deps = {}
        for b in R:
            if b.last_w is not None:
                deps[b.last_w.idx] = b.last_w
        for b in W:
            if b.last_w is not None:
                deps[b.last_w.idx] = b.last_w
            for r in b.readers:
                deps[r.idx] = r
        op.deps = []
        for d in deps.values():
            if d.eng == "pe" and eng == "pe" and d.dma is None and dma is None:
                continue
            d.signal = True
            op.deps.append(d)
        for b in R:
            b.readers.append(op)
        for b in W:
            b.last_w = op
            b.readers = []
        if dma is not None:
            op.signal = True
            n = self.n_dma.get(dma, 0) + 1
            self.n_dma[dma] = n
            op.semkey = "dma_" + dma
            op.sig = 16 * n
        self.ops.append(op)
        return op

    def mm(self, out, lhsT, rhs, start=True, stop=True, R=(), W=(), **kw):
        return self.add("pe", lambda e: e.matmul(out, lhsT, rhs, start=start, stop=stop, **kw), R, W)

    def tr(self, out, in_, ident, R=(), W=()):
        return self.add("pe", lambda e: e.transpose(out, in_, ident), R, W)

    def act(self, out, in_, func, R=(), W=(), **kw):
        return self.add("act", lambda e: e.activation(out, in_, func, **kw), R, W)

    def v(self, eng, name, *args, R=(), W=(), **kw):
        return self.add(eng, lambda e: getattr(e, name)(*args, **kw), R, W)

    def dma(self, out, in_, key, R=(), W=(), eng="sp", is_out=False, **kw):
        if is_out:
            self.out_dma_keys.add(key)
        return self.add(eng, lambda e: e.dma_start(out=out, in_=in_, **kw), R, W, dma=key)

    def emit(self):
        nc = self.nc
        engs = ["pe", "act", "dve", "pool", "sp"]
        cnt = {e: 0 for e in engs}
        for op in self.ops:
            if op.dma is None and op.signal:
                cnt[op.eng] += 1
                op.sig = cnt[op.eng]
                op.semkey = "eng_" + op.eng
        semkeys = sorted({op.semkey for op in self.ops if op.semkey is not None})
        sems = {k: self.stack.enter_context(nc.semaphore(k)) for k in semkeys}
        per = {e: [op for op in self.ops if op.eng == e] for e in engs}
        final = [(sems["dma_" + k], 16 * self.n_dma[k]) for k in sorted(self.out_dma_keys)]
        block = self.stack.enter_context(nc.Block())

        def run(e, name):
            known = {}
            for op in per[name]:
                need = {}
                for d in op.deps:
                    if known.get(d.semkey, 0) < d.sig:
                        need[d.semkey] = max(need.get(d.semkey, 0), d.sig)
                for k, val in need.items():
                    e.wait_ge(sems[k], val)
                    known[k] = val
                ins = op.fn(e)
                if op.signal:
                    ins.then_inc(sems[op.semkey], 16 if op.dma is not None else 1)
            if name == "sp":
                for s, val in final:
                    e.wait_ge(s, val)

        @block.tensor
        def _(e):
            run(e, "pe")

        @block.scalar
        def _(e):
            run(e, "act")

        @block.vector
        def _(e):
            run(e, "dve")

        @block.gpsimd
        def _(e):
            run(e, "pool")

        @block.sync
        def _(e):
            run(e, "sp")

    def close(self):
        self.stack.close()


class Rot:
    def __init__(self, items):
        self.items = items
        self.i = 0

    def next(self):
        it = self.items[self.i % len(self.items)]
        self.i += 1
        return it


def bc(ap, shape, axis):
    return ap.unsqueeze(axis).broadcast_to(list(shape))


A_TCH = ([(i, 128 * i, True, None) for i in range(4)]
         + [(4, 512, True, None)]
         + [(5 + i, 768 + 128 * i, True, 9 + i) for i in range(4)]
         + [(13, 1280, False, None), (14, 1408, False, None)]
         + [(15, 1536, True, None), (16, 1792, True, None)])
A_ROT_COLS = [(0, 512), (512, 128), (768, 512), (1536, 128), (1792, 128)]
A_VCH = [(640, 128), (1664, 128), (1920, 152), (2072, 512), (2584, 512), (3096, 512), (3608, 512)]
TWO_PI = 2.0 * np.pi
C1 = 6.28125
C2 = TWO_PI - C1


def build_A(ngroups=TOK // 512, do_rope=True, do_w=True, lvl=3, oeng="pool", addeng="pool", tch=None):
    nc = bass.Bass("TRN2", target_bir_lowering=False)
    x = nc.dram_tensor("x", [TOK, D], F32, kind="ExternalInput").ap()
    pos = nc.dram_tensor("pos", [1, TOK], I32, kind="ExternalInput").ap()
    w_in = nc.dram_tensor("w_in", [D, INC], F32, kind="ExternalInput").ap()
    gain = nc.dram_tensor("gain", [128, 8], F32, kind="ExternalInput").ap()
    invf = nc.dram_tensor("invf", [128, 1], F32, kind="ExternalInput").ap()
    identd = nc.dram_tensor("ident", [128, 128], F32, kind="ExternalInput").ap()
    OT = nc.dram_tensor("OT", [17, 128, TOK], BF16, kind="ExternalOutput").ap()
    OV = nc.dram_tensor("OV", [TOK, 390], BF16, kind="ExternalOutput").ap()
    OG = nc.dram_tensor("OG", [TOK, 2072], F32, kind="ExternalOutput").ap()
    p = Prog(nc)

    wb = p.sb("wb", [128, 8, INC], BF16); b_wb = Buf("wb")
    rotmap = {}
    off = 0
    for c0, n in A_ROT_COLS:
        rotmap[c0] = off
        off += n
    NR = off
    wr = p.sb("wr", [128, 8, NR], BF16); b_wr = Buf("wr")
    sinT = p.sb("sinT", [128, TOK], F32); b_sin = Buf("sin")
    cosT = p.sb("cosT", [128, TOK], F32); b_cos = Buf("cos")
    gain_sb = p.sb("gain_sb", [128, 8], F32); b_gain = Buf("gain")
    invf_sb = p.sb("invf_sb", [128, 1], F32); b_invf = Buf("invf")
    identf = p.sb("identf", [128, 128], F32); b_identf = Buf("identf")
    ident = p.sb("identb", [128, 128], BF16); b_ident = Buf("ident")

    p.dma(gain_sb[:], gain[:, :], "gain", W=[b_gain])
    p.dma(invf_sb[:], invf[:, :], "invf", W=[b_invf])
    p.dma(identf[:], identd[:, :], "identf", W=[b_identf])
    p.v("dve", "tensor_copy", ident[:], identf[:], R=[b_identf], W=[b_ident])

    SW = 1030
    stg = Rot([(p.sb(f"stg{i}", [128, SW], F32), Buf(f"stg{i}")) for i in range(2)])
    ci = 0
    for k in range(8 if do_w else 0):
        for q in range(4):
            st, bst = stg.next()
            p.dma(st[:], w_in[k * 128:(k + 1) * 128, q * SW:(q + 1) * SW], bst.name, W=[bst])
            eng = ["act", "dve", "pool"][ci % 3]
            ci += 1
            if eng == "act":
                p.act(wb[:, k, q * SW:(q + 1) * SW], st[:], AF.Copy, R=[bst], W=[b_wb])
            else:
                p.v(eng, "tensor_copy", wb[:, k, q * SW:(q + 1) * SW], st[:], R=[bst], W=[b_wb])
    for k in range(8):
        for c0, n in A_ROT_COLS:
            src = wb[:, k, c0:c0 + n].rearrange("p (h t d) -> p h t d", t=2, d=32)
            dst = wr[:, k, rotmap[c0]:rotmap[c0] + n].rearrange("p (h t d) -> p h t d", t=2, d=32)
            p.v("pool", "tensor_scalar", dst[:, :, 0, :], src[:, :, 1, :], -1.0, None, ALU.mult, R=[b_wb], W=[b_wr])
            p.v("pool", "tensor_copy", dst[:, :, 1, :], src[:, :, 0, :], R=[b_wb], W=[b_wr])

    RC = 512
    posi = p.sb("posi", [128, RC], I32); b_posi = Buf("posi")
    ang = p.sb("ang", [128, RC], F32); b_ang = Buf("ang")
    ki = p.sb("ki", [128, RC], I32); b_ki = Buf("ki")
    kf = p.sb("kf", [128, RC], F32); b_kf = Buf("kf")
    rr = p.sb("rr", [128, RC], F32); b_rr = Buf("rr")
    mk = p.sb("mk", [128, RC], F32); b_mk = Buf("mk")
    for c in range(TOK // RC if do_rope else 0):
        sl = slice(c * RC, (c + 1) * RC)
        p.dma(posi[:], pos[0:1, sl].broadcast_to([128, RC]), "posi", W=[b_posi])
        p.v("dve", "tensor_copy", ang[:], posi[:], R=[b_posi], W=[b_ang])
        p.v("dve", "tensor_scalar", ang[:], ang[:], invf_sb[:, 0:1], None, ALU.mult, R=[b_ang, b_invf], W=[b_ang])
        for tab, b_tab, shift in ((sinT, b_sin, 0.0), (cosT, b_cos, 0.5 * np.pi)):
            p.v("dve", "tensor_scalar", ki[:], ang[:], float(shift), 1.0 / TWO_PI, ALU.add, ALU.mult, R=[b_ang], W=[b_ki])
            p.v("dve", "tensor_copy", kf[:], ki[:], R=[b_ki], W=[b_kf])
            p.v("dve", "scalar_tensor_tensor", rr[:], kf[:], -C1, ang[:], ALU.mult, ALU.add, R=[b_kf, b_ang], W=[b_rr])
            p.v("dve", "scalar_tensor_tensor", rr[:], kf[:], -C2, rr[:], ALU.mult, ALU.add, R=[b_kf, b_rr], W=[b_rr])
            p.v("dve", "tensor_scalar", rr[:], rr[:], float(shift), None, ALU.add, R=[b_rr], W=[b_rr])
            p.v("dve", "tensor_scalar", mk[:], rr[:], -np.pi, TWO_PI, ALU.is_lt, ALU.mult, R=[b_rr], W=[b_mk])
            p.v("dve", "tensor_tensor", rr[:], rr[:], mk[:], ALU.add, R=[b_rr, b_mk], W=[b_rr])
            p.v("dve", "tensor_scalar", mk[:], rr[:], np.pi, -TWO_PI, ALU.is_gt, ALU.mult, R=[b_rr], W=[b_mk])
            p.v("dve", "tensor_tensor", rr[:], rr[:], mk[:], ALU.add, R=[b_rr, b_mk], W=[b_rr])
            p.v("dve", "tensor_scalar", rr[:], rr[:], 3.141592, -3.141592, ALU.min, ALU.max, R=[b_rr], W=[b_rr])
            p.act(tab[:, sl], rr[:], AF.Sin, R=[b_rr], W=[b_tab])

    xts = Rot([(p.sb(f"xt{i}", [128, D], F32), Buf(f"xt{i}")) for i in range(2)])
    xns = Rot([(p.sb(f"xn{i}", [128, D], BF16), Buf(f"xn{i}")) for i in range(2)])
    sts = Rot([(p.sb(f"st{i}", [128, 4], F32), Buf(f"stat{i}")) for i in range(2)])
    hT = p.sb("hT", [128, 8, 512], BF16); b_hT = Buf("hT")
    pst = p.ps("pst", [128, 8, 128], BF16); b_pst = Buf("pst")
    psU = Rot([(p.ps(f"psU{i}", [128, 512]), Buf(f"psU{i}")) for i in range(2)])
    psR = Rot([(p.ps(f"psR{i}", [128, 512]), Buf(f"psR{i}")) for i in range(2)])
    psV = Rot([(p.ps(f"psV{i}", [128, 512]), Buf(f"psV{i}")) for i in range(2)])
    t1s = Rot([(p.sb(f"t1_{i}", [128, 512], F32), Buf(f"t1_{i}")) for i in range(2)])
    t2s = Rot([(p.sb(f"t2_{i}", [128, 512], F32), Buf(f"t2_{i}")) for i in range(2)])
    obs = Rot([(p.sb(f"ob{i}", [128, 512], BF16), Buf(f"ob{i}")) for i in range(4)])
    ovs = Rot([(p.sb(f"ov{i}", [128, 390], BF16), Buf(f"ov{i}")) for i in range(2)])
    ogs = Rot([(p.sb(f"og{i}", [128, 2072], F32), Buf(f"og{i}")) for i in range(1)])
    for ov, b_ov in ovs.items:
        p.v("pool", "memset", ov[:], 1.0, W=[b_ov])

    for gi in range(ngroups):
        g0 = gi * 512
        for tl in range(4):
            r0 = g0 + tl * 128
            xt, b_xt = xts.next()
            xn, b_xn = xns.next()
            st, b_st = sts.next()
            p.dma(xt[:], x[r0:r0 + 128, :], b_xt.name, W=[b_xt])
            p.act(xn[:], xt[:], AF.Square, R=[b_xt], W=[b_xn, b_st], accum_out=st[:, 0:1])
            p.act(st[:, 1:2], st[:, 0:1], AF.Sqrt, R=[b_st], W=[b_st], scale=1.0 / D, bias=EPS)
            p.v("dve", "reciprocal", st[:, 2:3], st[:, 1:2], R=[b_st], W=[b_st])
            p.v("dve", "tensor_scalar", xn[:], xt[:], st[:, 2:3], None, ALU.mult, R=[b_xt, b_st], W=[b_xn])
            for k in range(8):
                p.tr(pst[:, k, :], xn[:, k * 128:(k + 1) * 128], ident[:], R=[b_xn, b_ident], W=[b_pst])
            p.v("dve", "tensor_tensor", hT[:, :, tl * 128:(tl + 1) * 128], pst[:, :, :],
                bc(gain_sb[:, :], [128, 8, 128], 2), ALU.mult, R=[b_pst, b_gain], W=[b_hT])
        for (oi, c0, rope, rawi) in ((A_TCH if tch is None else tch) if lvl >= 2 else []):
            pu, b_pu = psU.next()
            for k in range(8):
                p.mm(pu[:], wb[:, k, c0:c0 + 128], hT[:, k, :], start=(k == 0), stop=(k == 7), R=[b_wb, b_hT], W=[b_pu])
            if rope:
                rbase = max(c for c in rotmap if c <= c0)
                rc0 = rotmap[rbase] + (c0 - rbase)
                pr, b_pr = psR.next()
                for k in range(8):
                    p.mm(pr[:], wr[:, k, rc0:rc0 + 128], hT[:, k, :], start=(k == 0), stop=(k == 7), R=[b_wr, b_hT], W=[b_pr])
                t1, b_t1 = t1s.next()
                t2, b_t2 = t2s.next()
                p.v("dve", "tensor_tensor", t1[:], pu[:], cosT[:, g0:g0 + 512], ALU.mult, R=[b_pu, b_cos], W=[b_t1])
                p.v("dve", "tensor_tensor", t2[:], pr[:], sinT[:, g0:g0 + 512], ALU.mult, R=[b_pr, b_sin], W=[b_t2])
                ob, b_ob = obs.next()
                p.v(addeng, "tensor_tensor", ob[:], t1[:], t2[:], ALU.add, R=[b_t1, b_t2], W=[b_ob])
                p.dma(OT[oi, :, g0:g0 + 512], ob[:], b_ob.name, R=[b_ob], eng=oeng, is_out=True)
                if rawi is not None:
                    ob, b_ob = obs.next()
                    p.v("dve", "tensor_copy", ob[:], pu[:], R=[b_pu], W=[b_ob])
                    p.dma(OT[rawi, :, g0:g0 + 512], ob[:], b_ob.name, R=[b_ob], eng=oeng, is_out=True)
            else:
                ob, b_ob = obs.next()
                p.act(ob[:], pu[:], AF.Copy, R=[b_pu], W=[b_ob])
                p.dma(OT[oi, :, g0:g0 + 512], ob[:], b_ob.name, R=[b_ob], eng=oeng, is_out=True)
        for tl in range(4 if lvl >= 3 else 0):
            r0 = g0 + tl * 128
            ov, b_ov = ovs.next()
            og, b_og = ogs.next()
            ov4 = ov[:, :].rearrange("p (a g d) -> p a g d", a=3, g=2)
            for vi, (c0, n) in enumerate(A_VCH):
                pv, b_pv = psV.next()
                for k in range(8):
                    p.mm(pv[:, 0:n], hT[:, k, tl * 128:(tl + 1) * 128], wb[:, k, c0:c0 + n], start=(k == 0), stop=(k == 7),
                         R=[b_wb, b_hT], W=[b_pv])
                if vi < 3:
                    p.v("dve", "tensor_copy", ov4[:, vi, :, 0:64], pv[:, 0:128].rearrange("p (g d) -> p g d", g=2),
                        R=[b_pv], W=[b_ov])
                    if vi == 2:
                        p.act(og[:, 0:24], pv[:, 128:152], AF.Sigmoid, R=[b_pv], W=[b_og, b_pv])
                else:
                    o0 = 24 + (vi - 3) * 512
                    p.act(og[:, o0:o0 + 512], pv[:, 0:512], AF.Sigmoid, R=[b_pv], W=[b_og])
            p.dma(OV[r0:r0 + 128, :], ov[:], b_ov.name, R=[b_ov], eng=oeng, is_out=True)
            p.dma(OG[r0:r0 + 128, :], og[:], b_og.name, R=[b_og], eng=oeng, is_out=True)
    p.emit()
    p.close()
    return nc


NFC = 44


def load_cast(p, dst_fn, src_fn, nrow_chunks, ncols, stg, colw):
    ci = 0
    for k in range(nrow_chunks):
        for c0 in range(0, ncols, colw):
            c1 = min(ncols, c0 + colw)
            st, bst = stg.next()
            p.dma(st[:, 0:c1 - c0], src_fn(k, c0, c1), bst.name, W=[bst])
            eng = ["act", "dve", "pool"][ci % 3]
            ci += 1
            dst, bdst = dst_fn(k, c0, c1)
            if eng == "act":
                p.act(dst, st[:, 0:c1 - c0], AF.Copy, R=[bst], W=[bdst])
            else:
                p.v(eng, "tensor_copy", dst, st[:, 0:c1 - c0], R=[bst], W=[bdst])


def norm_T(p, xt, b_xt, npart, xn, b_xn, st, b_st, pst, b_pst, ident, b_ident, gain_sb, b_gain, hT_dst, b_hT):
    p.act(xn[0:npart, :], xt[0:npart, :], AF.Square, R=[b_xt], W=[b_xn, b_st], accum_out=st[0:npart, 0:1])
    p.act(st[0:npart, 1:2], st[0:npart, 0:1], AF.Sqrt, R=[b_st], W=[b_st], scale=1.0 / D, bias=EPS)
    p.v("dve", "reciprocal", st[0:npart, 2:3], st[0:npart, 1:2], R=[b_st], W=[b_st])
    p.v("dve", "tensor_scalar", xn[0:npart, :], xt[0:npart, :], st[0:npart, 2:3], None, ALU.mult, R=[b_xt, b_st], W=[b_xn])
    for k in range(8):
        p.tr(pst[:, k, 0:npart], xn[0:npart, k * 128:(k + 1) * 128], ident[0:npart, 0:npart], R=[b_xn, b_ident], W=[b_pst])
    p.v("dve", "tensor_tensor", hT_dst, pst[:, :, 0:npart], bc(gain_sb[:, :], [128, 8, npart], 2), ALU.mult,
        R=[b_pst, b_gain], W=[b_hT])


GS = 256


def build_C(ngroups=TOK // GS):
    nc = bass.Bass("TRN2", target_bir_lowering=False)
    xm = nc.dram_tensor("xm", [TOK, D], F32, kind="ExternalInput").ap()
    halo = nc.dram_tensor("halo", [2, D], F32, kind="ExternalInput").ap()
    w_up = nc.dram_tensor("w_up", [D, 2 * DFF], F32, kind="ExternalInput").ap()
    w_dn = nc.dram_tensor("w_dn", [DFF, D], F32, kind="ExternalInput").ap()
    cwd = nc.dram_tensor("cw", [128, NFC * 3], F32, kind="ExternalInput").ap()
    cbd = nc.dram_tensor("cb", [128, NFC], F32, kind="ExternalInput").ap()
    gain = nc.dram_tensor("gain", [128, 8], F32, kind="ExternalInput").ap()
    pgain = nc.dram_tensor("pgain", [1, D], F32, kind="ExternalInput").ap()
    identd = nc.dram_tensor("ident", [128, 128], F32, kind="ExternalInput").ap()
    xo = nc.dram_tensor("xo", [TOK, D], F32, kind="ExternalOutput").ap()
    p = Prog(nc)
    wub = p.sb("wub", [128, 8, 2 * DFF], BF16); b_wub = Buf("wub")
    wdb = p.sb("wdb", [128, 22, D], BF16); b_wdb = Buf("wdb")
    cw = p.sb("cw_sb", [128, NFC * 3], F32); b_cw = Buf("cw")
    cb = p.sb("cb_sb", [128, NFC], F32); b_cb = Buf("cb")
    gain_sb = p.sb("gain_sb", [128, 8], F32); b_gain = Buf("gain")
    pg = p.sb("pg", [128, D], F32); b_pg = Buf("pg")
    identf = p.sb("identf", [128, 128], F32); b_identf = Buf("identf")
    ident = p.sb("identb", [128, 128], BF16); b_ident = Buf("ident")
    p.dma(gain_sb[:], gain[:, :], "gain", W=[b_gain])
    p.dma(cw[:], cwd[:, :], "cw", W=[b_cw])
    p.dma(cb[:], cbd[:, :], "cb", W=[b_cb])
    p.dma(pg[:], pgain[0:1, :].broadcast_to([128, D]), "pg", W=[b_pg])
    p.dma(identf[:], identd[:, :], "identf", W=[b_identf])
    p.v("dve", "tensor_copy", ident[:], identf[:], R=[b_identf], W=[b_ident])
    SW = 512
    stg = Rot([(p.sb(f"stg{i}", [128, SW], F32), Buf(f"stg{i}")) for i in range(2)])
    load_cast(p, lambda k, c0, c1: (wub[:, k, c0:c1], b_wub), lambda k, c0, c1: w_up[k * 128:(k + 1) * 128, c0:c1], 8, 2 * DFF, stg, SW)
    load_cast(p, lambda k, c0, c1: (wdb[:, k, c0:c1], b_wdb), lambda k, c0, c1: w_dn[k * 128:(k + 1) * 128, c0:c1], 22, D, stg, SW)

    xts = [(p.sb(f"xt{i}", [128, D], F32), Buf(f"xt{i}")) for i in range(GS // 128)]
    xns = Rot([(p.sb(f"xn{i}", [128, D], BF16), Buf(f"xn{i}")) for i in range(2)])
    sts = Rot([(p.sb(f"st{i}", [128, 4], F32), Buf(f"stat{i}")) for i in range(2)])
    hT = p.sb("hT", [128, 8, GS], BF16); b_hT = Buf("hT")
    hTh = p.sb("hTh", [128, 8, 2], BF16); b_hTh = Buf("hTh")
    gT = p.sb("gT", [128, 22, GS], BF16); b_gT = Buf("gT")
    carry = p.sb("carry", [128, NFC, 2], F32)
    b_carry = [Buf(f"carry{c}") for c in range(NFC)]
    usbs = Rot([(p.sb(f"usb{i}", [128, GS + 2], F32), Buf(f"usb{i}")) for i in range(2)])
    accs = Rot([(p.sb(f"acc{i}", [128, GS], F32), Buf(f"acc{i}")) for i in range(4)])
    ys = Rot([(p.sb(f"y{i}", [128, D], F32), Buf(f"y{i}")) for i in range(2)])
    pst = p.ps("pst", [128, 8, 128], BF16); b_pst = Buf("pst")
    psu = Rot([(p.ps(f"psu{i}", [128, 512]), Buf(f"psu{i}")) for i in range(2)])
    psh = p.ps("psh", [128, 512]); b_psh = Buf("psh")
    psd = [(p.ps(f"psd{i}", [128, 512]), Buf(f"psd{i}")) for i in range(2)]

    xh, b_xh = xts[0]
    p.v("pool", "memset", xh[:], 0.0, W=[b_xh])
    p.dma(xh[0:2, :], halo[:, :], b_xh.name, R=[b_xh], W=[b_xh])
    xn, b_xn = xns.next()
    st, b_st = sts.next()
    norm_T(p, xh, b_xh, 128, xn, b_xn, st, b_st, pst, b_pst, ident, b_ident, gain_sb, b_gain, hT[:, :, 0:128], b_hT)
    p.v("dve", "tensor_copy", hTh[:, :, :], hT[:, :, 0:2], R=[b_hT], W=[b_hTh])
    for ch in range(NFC):
        for k in range(8):
            p.mm(psh[:, 2 * ch:2 * ch + 2], wub[:, k, ch * 128:(ch + 1) * 128], hTh[:, k, :], start=(k == 0), stop=(k == 7),
                 R=[b_wub, b_hTh], W=[b_psh])
    p.v("dve", "tensor_copy", carry[:, :, :], psh[:, 0:2 * NFC].rearrange("p (c t) -> p c t", t=2), R=[b_psh], W=b_carry)

    for gi in range(ngroups):
        g0 = gi * GS
        for tl in range(GS // 128):
            r0 = g0 + tl * 128
            xt, b_xt = xts[tl]
            xn, b_xn = xns.next()
            st, b_st = sts.next()
            p.dma(xt[:], xm[r0:r0 + 128, :], b_xt.name, W=[b_xt])
            norm_T(p, xt, b_xt, 128, xn, b_xn, st, b_st, pst, b_pst, ident, b_ident, gain_sb, b_gain,
                   hT[:, :, tl * 128:(tl + 1) * 128], b_hT)
        for c in range(22):
            accl = []
            for half, ch in enumerate((c, c + 22)):
                pu, b_pu = psu.next()
                for k in range(8):
                    p.mm(pu[:, 0:GS], wub[:, k, ch * 128:(ch + 1) * 128], hT[:, k, :], start=(k == 0), stop=(k == 7), R=[b_wub, b_hT], W=[b_pu])
                us, b_us = usbs.next()
                p.act(us[:, 2:GS + 2], pu[:, 0:GS], AF.Copy, R=[b_pu], W=[b_us])
                p.v("pool", "tensor_copy", us[:, 0:2], carry[:, ch, :], R=[b_carry[ch]], W=[b_us])
                p.v("pool", "tensor_copy", carry[:, ch, :], us[:, GS:GS + 2], R=[b_us], W=[b_carry[ch]])
                ac, b_ac = accs.next()
                eng = "dve"
                p.v("dve", "tensor_scalar", ac[:], us[:, 2:GS + 2], cw[:, 3 * ch + 2:3 * ch + 3], cb[:, ch:ch + 1], ALU.mult, ALU.add,
                    R=[b_us, b_cw, b_cb], W=[b_ac])
                p.v(eng, "scalar_tensor_tensor", ac[:], us[:, 1:GS + 1], cw[:, 3 * ch + 1:3 * ch + 2], ac[:], ALU.mult, ALU.add,
                    R=[b_us, b_cw, b_ac], W=[b_ac])
                p.v(eng, "scalar_tensor_tensor", ac[:], us[:, 0:GS], cw[:, 3 * ch:3 * ch + 1], ac[:], ALU.mult, ALU.add,
                    R=[b_us, b_cw, b_ac], W=[b_ac])
                accl.append((ac, b_ac))
            (aa, b_aa), (av, b_av) = accl
            p.act(aa[:], aa[:], AF.Gelu_apprx_tanh, R=[b_aa], W=[b_aa])
            p.v("dve", "tensor_tensor", gT[:, c, :], aa[:], av[:], ALU.mult, R=[b_aa, b_av], W=[b_gT])
        for tl in range(GS // 128):
            r0 = g0 + tl * 128
            xt, b_xt = xts[tl]
            st, b_st = sts.next()
            y, b_y = ys.next()
            for hh in range(2):
                pd, b_pd = psd[hh]
                for c in range(22):
                    p.mm(pd[:], gT[:, c, tl * 128:(tl + 1) * 128], wdb[:, c, hh * 512:(hh + 1) * 512], start=(c == 0), stop=(c == 21),
                         R=[b_gT, b_wdb], W=[b_pd])
                p.act(y[:, hh * 512:(hh + 1) * 512], pd[:], AF.Square, R=[b_pd], W=[b_y, b_st], accum_out=st[:, hh:hh + 1])
            p.v("dve", "tensor_tensor", st[:, 2:3], st[:, 0:1], st[:, 1:2], ALU.add, R=[b_st], W=[b_st])
            p.act(st[:, 3:4], st[:, 2:3], AF.Sqrt, R=[b_st], W=[b_st], scale=1.0 / D, bias=EPS)
            p.v("dve", "reciprocal", st[:, 2:3], st[:, 3:4], R=[b_st], W=[b_st])
            for hh in range(2):
                pd, b_pd = psd[hh]
                p.v("dve", "scalar_tensor_tensor", y[:, hh * 512:(hh + 1) * 512], pd[:], st[:, 2:3], pg[:, hh * 512:(hh + 1) * 512],
                    ALU.mult, ALU.mult, R=[b_pd, b_st, b_pg, b_y], W=[b_y])
            p.v("pool", "tensor_tensor", y[:], y[:], xt[:], ALU.add, R=[b_y, b_xt], W=[b_y])
            p.dma(xo[r0:r0 + 128, :], y[:], b_y.name, R=[b_y], eng="pool", is_out=True)
    p.emit()
    p.close()
    return nc


def run_C(nc, xm_cores, halo_cores, w_up, w_dn, conv_w, conv_b, pre_gain, post_gain):
    cst = const_inputs()
    cw = np.ascontiguousarray(conv_w.T.reshape(NFC, 128, 3).transpose(1, 0, 2).reshape(128, NFC * 3)).astype(np.float32)
    cbv = np.ascontiguousarray(conv_b.reshape(NFC, 128).T).astype(np.float32)
    maps = []
    for c in range(NCORES):
        maps.append({"xm": xm_cores[c], "halo": halo_cores[c], "w_up": w_up, "w_dn": w_dn, "cw": cw, "cb": cbv,
                     "gain": gain_layout(pre_gain), "pgain": post_gain.reshape(1, D).astype(np.float32), "ident": cst["ident"]})
    res = run_bass_kernel_spmd(nc, maps, core_ids=list(range(NCORES)))
    return res.results


NEG = -30000.0


def build_B(nblocks=NB):
    nc = bass.Bass("TRN2", target_bir_lowering=False)
    dI = lambda n, s, t: nc.dram_tensor(n, s, t, kind="ExternalInput").ap()
    QQd = dI("QQ", [NB, 128, 1536], BF16)
    KCd = dI("KC", [NB, 128, 1024], BF16)
    VCd = dI("VC", [NB, 128, 1040], BF16)
    KSFd = dI("KSF", [128, SEQ], BF16)
    VSFd = dI("VSF", [128, 128 * 130], BF16)
    KCBd = dI("KCBF", [128, SEQ + 16], BF16)
    VCBd = dI("VCBF", [128, SEQ + 16], BF16)
    OGd = dI("OGB", [NB, 128, 2072], F32)
    XBd = dI("XB", [NB, 128, D], F32)
    TQCd = dI("TQC", [128, 96], F32)
    TQRd = dI("TQR", [NB, 128], F32)
    CSTd = dI("CST", [128, 1552], F32)
    MSKd = dI("MSK", [128, 256], F32)
    EXd = dI("EX", [128, 8192], BF16)
    W1d = dI("W1", [2, 128, 4096], F32)
    POSTd = dI("POST", [64, 64], F32)
    B1d = dI("B1", [128, 2], F32)
    W2d = dI("W2", [128, 128], F32)
    WAd = dI("WA", [512, D], F32)
    WBd = dI("WB", [512, D], F32)
    WOd = dI("WO", [D, D], F32)
    PGd = dI("PG", [1, D], F32)
    SINKd = dI("SINK", [1, 8], F32)
    identd = dI("ident", [128, 128], F32)
    XMd = nc.dram_tensor("XM", [NB, 128, D], F32, kind="ExternalOutput").ap()
    p = Prog(nc)
    T = lambda n, s, t: (p.sb(n, s, t), Buf(n))

    big, b_big = T("big", [128, SEQ + 16], BF16)
    vsf, b_vsf = T("vsf", [128, 128 * 130], BF16)
    ex, b_ex = T("ex", [128, 8192], BF16)
    wab, b_wab = T("wab", [128, 8, D], BF16)
    wob, b_wob = T("wob", [128, 8, D], BF16)
    kcmpT, b_kcmpT = T("kcmpT", [128, 1024], BF16)
    vcmp, b_vcmp = T("vcmp", [128, 8, 130], BF16)
    cst, b_cst = T("cst", [128, 1552], F32)
    tqc, b_tqc = T("tqc", [128, 96], F32)
    mskf, b_mskf = T("mskf", [128, 256], F32)
    msk, b_msk = T("msk", [128, 256], BF16)
    pg, b_pg = T("pgb", [128, D], F32)
    esink, b_esink = T("esink", [128, 8], F32)
    identf, b_identf = T("identf", [128, 128], F32)
    ident, b_ident = T("identb", [128, 128], BF16)
    post, b_post = T("post", [64, 64], F32)
    postb, b_postb = T("postb", [64, 64], BF16)
    b1, b_b1 = T("b1", [128, 2], F32)
    beff, b_beff = T("beff", [128, 2], F32)
    w2f, b_w2f = T("w2f", [128, 128], F32)
    w2b, b_w2b = T("w2b", [128, 128], BF16)
    w2pad, b_w2pad = T("w2pad", [128, 2, 128], BF16)
    hid, b_hid = T("hid", [128, 2, 512], BF16)
    cmpend_row = cst[:, 0:1024]
    blk64 = cst[:, 1024:1280]
    b0row = cst[:, 1280:1536]
    cmpend_col = cst[:, 1536:1544]

    p.dma(cst[:], CSTd[:, :], "cst", W=[b_cst])
    p.dma(tqc[:], TQCd[:, :], "tqc", W=[b_tqc])
    p.dma(mskf[:], MSKd[:, :], "mskf", W=[b_mskf])
    p.v("dve", "tensor_copy", msk[:], mskf[:], R=[b_mskf], W=[b_msk])
    p.dma(pg[:], PGd[0:1, :].broadcast_to([128, D]), "pg", W=[b_pg])
    p.dma(esink[:], SINKd[0:1, :].broadcast_to([128, 8]), "esink", W=[b_esink])
    p.act(esink[:], esink[:], AF.Exp, R=[b_esink], W=[b_esink])
    p.dma(identf[:], identd[:, :], "identf", W=[b_identf])
    p.v("dve", "tensor_copy", ident[:], identf[:], R=[b_identf], W=[b_ident])
    p.dma(post[:], POSTd[:, :], "post", W=[b_post])
    p.v("dve", "tensor_copy", postb[:], post[:], R=[b_post], W=[b_postb])
    p.dma(b1[:], B1d[:, :], "b1", W=[b_b1])
    p.dma(w2f[:], W2d[:, :], "w2f", W=[b_w2f])
    p.v("dve", "tensor_copy", w2b[:], w2f[:], R=[b_w2f], W=[b_w2b])
    p.v("pool", "memset", w2pad[:], 0.0, W=[b_w2pad])
    p.v("dve", "tensor_copy", w2pad[:, 0, 0:64], w2f[:, 0:64], R=[b_w2f, b_w2pad], W=[b_w2pad])
    p.v("dve", "tensor_copy", w2pad[:, 1, 64:128], w2f[:, 0:64], R=[b_w2f, b_w2pad], W=[b_w2pad])
    p.dma(ex[:], EXd[:, :], "ex", W=[b_ex])

    SW = 1024
    stg = Rot([(p.sb(f"stg{i}", [128, SW], F32), Buf(f"stg{i}")) for i in range(2)])
    load_cast(p, lambda k, c0, c1: (vsf[:, k * 4096 + c0:k * 4096 + c1], b_vsf), lambda k, c0, c1: W1d[k, :, c0:c1], 2, 4096, stg, SW)
    load_cast(p, lambda k, c0, c1: (wab[:, k, c0:c1], b_wab), lambda k, c0, c1: WAd[k * 128:(k + 1) * 128, c0:c1], 4, D, stg, SW)
    load_cast(p, lambda k, c0, c1: (wab[:, 4 + k, c0:c1], b_wab), lambda k, c0, c1: WBd[k * 128:(k + 1) * 128, c0:c1], 4, D, stg, SW)
    load_cast(p, lambda k, c0, c1: (wob[:, k, c0:c1], b_wob), lambda k, c0, c1: WOd[k * 128:(k + 1) * 128, c0:c1], 8, D, stg, SW)

    ps_s = Rot([(p.ps(f"ps_s{i}", [128, 4, 128]), Buf(f"ps_s{i}")) for i in range(2)])
    ps_o = [(p.ps(f"ps_o{i}", [128, 4, 128]), Buf(f"ps_o{i}")) for i in range(2)]
    ps_c, b_ps_c = p.ps("ps_c", [128, 1024]), Buf("ps_c")
    ps_t, b_ps_t = p.ps("ps_t", [128, 8, 128], BF16), Buf("ps_t")
    ps_y, b_ps_y = p.ps("ps_y", [128, 512]), Buf("ps_y")

    w1v = vsf[:, 0:8192].rearrange("p (k j h) -> p k j h", k=2, j=32)
    for kv in range(2):
        p.dma(big[:], (KCBd if kv == 0 else VCBd)[:, :], "big", W=[b_big])
        for j in range(32):
            p.mm(ps_y[:, 0:1], w1v[0:64, kv, j, :], postb[:, kv * 32 + j:kv * 32 + j + 1], start=(j == 0), stop=(j == 31),
                 R=[b_vsf, b_postb], W=[b_ps_y])
        p.v("dve", "tensor_tensor", beff[:, kv:kv + 1], ps_y[:, 0:1], b1[:, kv:kv + 1], ALU.add, R=[b_ps_y, b_b1], W=[b_beff])
        for hf in range(2):
            c0 = hf * 512
            for g in range(2):
                rows = slice(g * 64, (g + 1) * 64)
                for j in range(32):
                    p.mm(ps_c[:, 0:512], w1v[rows, kv, j, :], big[rows, 16 * c0 + j:16 * c0 + j + 16 * 511 + 1:16],
                         start=(j == 0), stop=(j == 31), R=[b_vsf, b_big], W=[b_ps_c])
                p.act(hid[:, g, :], ps_c[:, 0:512], AF.Gelu_apprx_tanh, R=[b_ps_c, b_beff], W=[b_hid], bias=beff[:, kv:kv + 1])
            if kv == 0:
                for g in range(2):
                    p.mm(ps_y[:, :], w2pad[:, g, :], hid[:, g, :], start=(g == 0), stop=(g == 1), R=[b_w2pad, b_hid], W=[b_ps_y])
                p.v("dve", "tensor_copy", kcmpT[:, c0:c0 + 512], ps_y[:, :], R=[b_ps_y], W=[b_kcmpT])
            else:
                for ct in range(4):
                    for g in range(2):
                        p.mm(ps_y[:, g * 64:(g + 1) * 64], hid[:, g, ct * 128:(ct + 1) * 128], w2b[:, 64:128], start=True, stop=True,
                             R=[b_hid, b_w2b], W=[b_ps_y])
                    p.v("dve", "tensor_copy", vcmp[:, hf * 4 + ct, :].rearrange("p (g e) -> p g e", g=2)[:, :, 0:64],
                        ps_y[:, 0:128].rearrange("p (g d) -> p g d", g=2), R=[b_ps_y, b_vcmp], W=[b_vcmp])
    vc4 = vcmp[:, :, :].rearrange("p k (g e) -> p k g e", g=2)
    p.v("pool", "memset", vc4[:, :, :, 64:65], 1.0, R=[b_vcmp], W=[b_vcmp])
    p.dma(big[:, 0:SEQ], KSFd[:, :], "big", W=[b_big])
    p.dma(vsf[:], VSFd[:, :], "vsf", W=[b_vsf])

    qq, b_qq = T("qq", [128, 3, 4, 128], BF16)
    kc, b_kc = T("kc", [128, 8, 128], BF16)
    vc, b_vc = T("vc", [128, 8, 130], BF16)
    ogb, b_ogb = T("ogb", [128, 2072], F32)
    xb, b_xb = T("xb", [128, D], F32)
    tqr, b_tqr = T("tqr", [128, 128], F32)
    ec, b_ec = T("ec", [128, 1024], F32)
    mkc, b_mkc = T("mkc", [128, 1024], F32)
    imp, b_imp = T("imp", [128, 1024], F32)
    mct, b_mct = T("mct", [128, 128], BF16)
    sv = {n: T("sv_" + n, [128, 256], F32) for n in ("valid", "bonus", "vm1", "notown", "imps", "v", "v2", "sel")}
    biasb, b_biasb = T("biasb", [128, 256], BF16)
    biasT4, b_biasT4 = T("biasT4", [128, 2, 4, 128], BF16)
    pTs = Rot([T(f"pT{i}", [128, 4, 128], BF16) for i in range(4)])
    st, b_st = T("stt", [128, 64], F32)
    oa, b_oa = T("oa", [128, 8, 64], BF16)
    ob32, b_ob32 = T("ob32", [128, 8, 64], F32)
    otmp, b_otmp = T("otmp", [128, 8, 64], F32)
    obb, b_obb = T("obb", [128, 8, 64], BF16)
    oT, b_oT = T("oT", [128, 8, 128], BF16)
    y32, b_y32 = T("y32", [128, D], F32)
    mixb, b_mixb = T("mixb", [128, D], BF16)
    p.v("pool", "memset", biasb[:], 0.0, W=[b_biasb])
    acc = p.sb("acc", [128, 2, 4, 65], F32)
    b_acc = [Buf("acc0"), Buf("acc1")]

    def accum(g, first, pi=None):
        po, b_po = ps_o[g if pi is None else pi]
        if first:
            p.v("dve", "tensor_copy", acc[:, g, :, :], po[:, :, 0:65], R=[b_po], W=[b_acc[g]])
        else:
            p.v("dve", "tensor_tensor", acc[:, g, :, :], acc[:, g, :, :], po[:, :, 0:65], ALU.add, R=[b_po, b_acc[g]], W=[b_acc[g]])

    def branch(tiles, qm, po_list):
        nt = len(tiles)
        for ti, (kfn, vfn, mask, bias) in enumerate(tiles):
            for g in range(2):
                rows = slice(g * 64, (g + 1) * 64)
                ps, b_ps = ps_s.next()
                kap, kbufs = kfn(g)
                p.mm(ps[:, :, :], kap, qq[rows, qm, :, :], start=True, stop=(bias is None), R=kbufs + [b_qq], W=[b_ps])
                if bias is not None:
                    lap, rap = bias
                    p.mm(ps[:, :, :], lap, rap, start=False, stop=True, R=[b_ex, b_biasT4], W=[b_ps])
                pT, b_pT = pTs.next()
                p.act(pT[:, :, :], ps[:, :, :], AF.Exp, R=[b_ps], W=[b_pT], scale=0.125)
                if mask is not None:
                    map_, mbufs = mask
                    p.v("pool", "tensor_tensor", pT[:, :, :], pT[:, :, :], bc(map_, [128, 4, 128], 1), ALU.mult, R=[b_pT] + mbufs, W=[b_pT])
                vap, vbufs = vfn(g)
                po, b_po = po_list[g]
                for hh in range(4):
                    p.mm(po[:, hh, 0:65], pT[:, hh, :], vap, start=True, stop=True, R=[b_pT] + vbufs, W=[b_po])
                accum(g, ti == 0)

    def normalize(dst_ap, dst_bufs, gate_col0, extra_den, first, eng="dve"):
        for g in range(2):
            po, b_po = ps_o[g]
            den = st[:, 8 + 4 * g:12 + 4 * g]
            p.v("dve", "tensor_copy", den, acc[:, g, :, 64], R=[b_acc[g], b_st], W=[b_st])
            if extra_den is not None:
                p.v("dve", "tensor_tensor", den, den, extra_den[:, 4 * g:4 * g + 4], ALU.add, R=[b_st, b_esink], W=[b_st])
            p.v("dve", "tensor_scalar", den, den, 1e-30, None, ALU.max, R=[b_st], W=[b_st])
            p.v("dve", "reciprocal", den, den, R=[b_st], W=[b_st])
            if gate_col0 is not None:
                gv = ogb[:, 12 * g:12 * g + 12].rearrange("p (h t) -> p h t", t=3)[:, :, gate_col0]
                p.v("dve", "tensor_tensor", den, den, gv, ALU.mult, R=[b_st, b_ogb], W=[b_st])
            tgt = dst_ap[:, 4 * g:4 * g + 4, :] if first else otmp[:, 4 * g:4 * g + 4, :]
            p.v("dve", "tensor_tensor", tgt, acc[:, g, :, 0:64], bc(den, [128, 4, 64], 2), ALU.mult,
                R=[b_acc[g], b_st], W=(dst_bufs if first else [b_otmp]))
            if not first:
                p.v("pool", "tensor_tensor", dst_ap[:, 4 * g:4 * g + 4, :], dst_ap[:, 4 * g:4 * g + 4, :], otmp[:, 4 * g:4 * g + 4, :], ALU.add,
                    R=dst_bufs + [b_otmp], W=dst_bufs)

    kcf = lambda t: (lambda g: (kc[g * 64:(g + 1) * 64, t, :], [b_kc]))
    vcf = lambda t: (lambda g: (vc[:, t, g * 65:(g + 1) * 65], [b_vc]))
    mD = (msk[:, 0:128], [b_msk])
    mP = (msk[:, 128:256], [b_msk])

    for j in range(nblocks):
        p.dma(qq[:, :, :, :].rearrange("p a h q -> p (a h q)"), QQd[j], "qq", W=[b_qq])
        p.dma(kc[:, :, :].rearrange("p t k -> p (t k)"), KCd[j], "kc", W=[b_kc])
        p.dma(vc[:, :, :].rearrange("p t k -> p (t k)"), VCd[j], "vc", W=[b_vc])
        p.dma(ogb[:], OGd[j], "ogb", W=[b_ogb])
        p.dma(xb[:], XBd[j], "xb", W=[b_xb])
        p.dma(tqr[:], TQRd[j:j + 1, :].broadcast_to([128, 128]), "tqr", W=[b_tqr])
        tq_col = tqc[:, j:j + 1]
        tm128_col = tqc[:, 32 + j:33 + j]
        t0_col = tqc[:, 64 + j:65 + j]
        branch([(kcf(0), vcf(0), mP, None), (kcf(1), vcf(1), mD, None)], 0, ps_o)
        normalize(oa, [b_oa], None, esink, True)
        branch([(kcf(2), vcf(2), mP, None), (kcf(3), vcf(3), None, None), (kcf(4), vcf(4), None, None),
                (kcf(5), vcf(5), None, None), (kcf(6), vcf(6), mD, None)], 1, ps_o)
        normalize(ob32, [b_ob32], 2, None, True)
        nkt = (4 * j + 3) // 16 + 1
        ncols = 128 * nkt
        nblk = 8 * j + 8
        p.v("dve", "tensor_scalar", mkc[:, 0:ncols], cmpend_row[:, 0:ncols], tq_col, None, ALU.is_le, R=[b_cst, b_tqc], W=[b_mkc])
        va, b_va = sv["valid"]; bo, b_bo = sv["bonus"]; vm1, b_vm1 = sv["vm1"]; no, b_no = sv["notown"]
        p.v("dve", "tensor_scalar", va[:, 0:nblk], blk64[:, 0:nblk], tq_col, None, ALU.is_le, R=[b_cst, b_tqc], W=[b_va])
        p.v("dve", "tensor_scalar", bo[:, 0:nblk], blk64[:, 0:nblk], tm128_col, 100.0, ALU.is_gt, ALU.mult, R=[b_cst, b_tqc], W=[b_bo])
        p.v("dve", "tensor_tensor", bo[:, 0:nblk], bo[:, 0:nblk], b0row[:, 0:nblk], ALU.add, R=[b_bo, b_cst], W=[b_bo])
        p.v("dve", "tensor_scalar", vm1[:, 0:nblk], va[:, 0:nblk], -1.0, None, ALU.add, R=[b_va], W=[b_vm1])
        p.v("dve", "tensor_scalar", no[:, 0:nblk], blk64[:, 0:nblk], t0_col, None, ALU.is_lt, R=[b_cst, b_tqc], W=[b_no])
        sel_bias = []
        for g in range(2):
            rows = slice(g * 64, (g + 1) * 64)
            for hh in range(4):
                for c0 in range(0, ncols, 512):
                    c1 = min(ncols, c0 + 512)
                    p.mm(ps_c[:, c0:c1], qq[rows, 2, hh, :], kcmpT[rows, c0:c1], start=True, stop=True, R=[b_qq, b_kcmpT], W=[b_ps_c])
                p.act(ec[:, 0:ncols], ps_c[:, 0:ncols], AF.Exp, R=[b_ps_c], W=[b_ec], scale=0.125)
                dcol = st[:, 16 + hh:17 + hh]
                p.v("dve", "scalar_tensor_tensor", ec[:, 0:ncols], ec[:, 0:ncols], 1.0, mkc[:, 0:ncols], ALU.mult, ALU.mult,
                    R=[b_ec, b_mkc, b_st], W=[b_ec, b_st], accum_out=dcol)
                p.v("dve", "tensor_scalar", dcol, dcol, 1e-30, None, ALU.max, R=[b_st], W=[b_st])
                p.v("dve", "reciprocal", dcol, dcol, R=[b_st], W=[b_st])
                if hh == 0:
                    p.v("dve", "tensor_scalar", imp[:, 0:ncols], ec[:, 0:ncols], dcol, None, ALU.mult, R=[b_ec, b_st], W=[b_imp])
                else:
                    p.v("dve", "scalar_tensor_tensor", imp[:, 0:ncols], ec[:, 0:ncols], dcol, imp[:, 0:ncols], ALU.mult, ALU.add,
                        R=[b_ec, b_st, b_imp], W=[b_imp])
            iv = imp[:, 0:4 * nblk].rearrange("p (b f) -> p b f", f=4)
            ims, b_ims = sv["imps"]; vv, b_vv = sv["v"]; v2, b_v2 = sv["v2"]; se, b_se = sv["sel"]
            p.v("dve", "tensor_tensor", ims[:, 0:nblk], iv[:, :, 0], iv[:, :, 1], ALU.add, R=[b_imp], W=[b_ims])
            p.v("dve", "tensor_tensor", ims[:, 0:nblk], ims[:, 0:nblk], iv[:, :, 2], ALU.add, R=[b_imp, b_ims], W=[b_ims])
            p.v("dve", "scalar_tensor_tensor", ims[:, 0:nblk], ims[:, 0:nblk], 2.0, iv[:, :, 3], ALU.mult, ALU.add, R=[b_imp, b_ims], W=[b_ims])
            p.v("dve", "tensor_tensor", vv[:, 0:nblk], ims[:, 0:nblk], bo[:, 0:nblk], ALU.add, R=[b_ims, b_bo], W=[b_vv])
            p.v("dve", "tensor_tensor", vv[:, 1:nblk], vv[:, 1:nblk], iv[:, 0:nblk - 1, 3], ALU.add, R=[b_imp, b_vv], W=[b_vv])
            p.v("dve", "tensor_tensor", vv[:, 0:nblk], vv[:, 0:nblk], va[:, 0:nblk], ALU.mult, R=[b_vv, b_va], W=[b_vv])
            p.v("dve", "tensor_tensor", vv[:, 0:nblk], vv[:, 0:nblk], vm1[:, 0:nblk], ALU.add, R=[b_vv, b_vm1], W=[b_vv])
            p.v("dve", "max", st[:, 24:32], vv[:, 0:nblk], R=[b_vv, b_st], W=[b_st])
            p.v("dve", "match_replace", v2[:, 0:nblk], st[:, 24:32], vv[:, 0:nblk], -2.0, R=[b_vv, b_st], W=[b_v2])
            p.v("dve", "max", st[:, 32:40], v2[:, 0:nblk], R=[b_v2, b_st], W=[b_st])
            p.v("dve", "tensor_scalar", se[:, 0:nblk], vv[:, 0:nblk], st[:, 39:40], None, ALU.is_ge, R=[b_vv, b_st], W=[b_se])
            p.v("dve", "tensor_tensor", se[:, 0:nblk], se[:, 0:nblk], va[:, 0:nblk], ALU.mult, R=[b_se, b_va], W=[b_se])
            p.v("dve", "tensor_tensor", se[:, 0:nblk], se[:, 0:nblk], no[:, 0:nblk], ALU.mult, R=[b_se, b_no], W=[b_se])
            p.v("dve", "tensor_scalar", biasb[:, 0:nblk], se[:, 0:nblk], -1.0, -NEG, ALU.add, ALU.mult, R=[b_se, b_biasb], W=[b_biasb])
            nch = (nblk + 127) // 128
            for ch in range(nch):
                p.tr(ps_t[:, ch, :], biasb[:, ch * 128:(ch + 1) * 128], ident[:, :], R=[b_biasb, b_ident], W=[b_ps_t])
            sel_bias.append(nch)
            for ch in range(nch):
                p.v("dve", "tensor_copy", biasT4[:, ch, :, :], bc(ps_t[:, ch, :], [128, 4, 128], 1), R=[b_ps_t, b_biasT4], W=[b_biasT4])
            ntile = 4 * j + 3
            for kt in range(ntile + 1):
                po, b_po = ps_o[kt % 2]
                diag = (kt == ntile)
                ps, b_ps = ps_s.next()
                if diag:
                    p.mm(ps[:, :, :], kc[rows, 7, :], qq[rows, 1, :, :], start=True, stop=True, R=[b_kc, b_qq], W=[b_ps])
                else:
                    p.mm(ps[:, :, :], big[rows, kt * 128:(kt + 1) * 128], qq[rows, 1, :, :], start=True, stop=False, R=[b_big, b_qq], W=[b_ps])
                    p.mm(ps[:, :, :], ex[:, (kt % 64) * 128:(kt % 64 + 1) * 128], biasT4[:, kt // 64, :, :], start=False, stop=True,
                         R=[b_ex, b_biasT4], W=[b_ps])
                pT, b_pT = pTs.next()
                p.act(pT[:, :, :], ps[:, :, :], AF.Exp, R=[b_ps], W=[b_pT], scale=0.125)
                if diag:
                    p.v("pool", "tensor_tensor", pT[:, :, :], pT[:, :, :], bc(msk[:, 0:128], [128, 4, 128], 1), ALU.mult, R=[b_pT, b_msk], W=[b_pT])
                    vap = vc[:, 7, g * 65:(g + 1) * 65]
                    vb = b_vc
                else:
                    vap = vsf[:, kt * 130 + g * 65:kt * 130 + (g + 1) * 65]
                    vb = b_vsf
                for hh in range(4):
                    p.mm(po[:, hh, 0:65], pT[:, hh, :], vap, start=True, stop=True, R=[b_pT, vb], W=[b_po])
                accum(g, kt == 0, kt % 2)
        normalize(ob32, [b_ob32], 1, None, False)
        tiles = []
        for kt in range(nkt):
            p.v("dve", "tensor_scalar", mct[:, :], tqr[:, :], cmpend_col[:, kt:kt + 1], None, ALU.is_ge, R=[b_tqr, b_cst, b_mct], W=[b_mct])
            branch([((lambda g, kt=kt: (kcmpT[g * 64:(g + 1) * 64, kt * 128:(kt + 1) * 128], [b_kcmpT])),
                     (lambda g, kt=kt: (vcmp[:, kt, g * 65:(g + 1) * 65], [b_vcmp])), (mct[:, :], [b_mct]), None)], 2, ps_o) if False else None
            for g in range(2):
                rows = slice(g * 64, (g + 1) * 64)
                ps, b_ps = ps_s.next()
                p.mm(ps[:, :, :], kcmpT[rows, kt * 128:(kt + 1) * 128], qq[rows, 2, :, :], start=True, stop=True, R=[b_kcmpT, b_qq], W=[b_ps])
                pT, b_pT = pTs.next()
                p.act(pT[:, :, :], ps[:, :, :], AF.Exp, R=[b_ps], W=[b_pT], scale=0.125)
                p.v("pool", "tensor_tensor", pT[:, :, :], pT[:, :, :], bc(mct[:, :], [128, 4, 128], 1), ALU.mult, R=[b_pT, b_mct], W=[b_pT])
                po, b_po = ps_o[g]
                for hh in range(4):
                    p.mm(po[:, hh, 0:65], pT[:, hh, :], vcmp[:, kt, g * 65:(g + 1) * 65], start=True, stop=True,
                         R=[b_pT, b_vcmp], W=[b_po])
                accum(g, kt == 0)
        normalize(ob32, [b_ob32], 0, None, False)
        p.v("dve", "tensor_copy", obb[:, :, :], ob32[:, :, :], R=[b_ob32], W=[b_obb])
        oaf = oa[:, :, :].rearrange("p h d -> p (h d)")
        obf = obb[:, :, :].rearrange("p h d -> p (h d)")
        for k in range(4):
            p.tr(ps_t[:, k, :], oaf[:, k * 128:(k + 1) * 128], ident[:, :], R=[b_oa, b_ident], W=[b_ps_t])
            p.tr(ps_t[:, 4 + k, :], obf[:, k * 128:(k + 1) * 128], ident[:, :], R=[b_obb, b_ident], W=[b_ps_t])
        p.v("dve", "tensor_copy", oT[:, :, :], ps_t[:, :, :], R=[b_ps_t], W=[b_oT])
        for hh in range(2):
            cs = slice(hh * 512, (hh + 1) * 512)
            for br in range(2):
                for k in range(4):
                    p.mm(ps_y[:, :], oT[:, 4 * br + k, :], wab[:, 4 * br + k, cs], start=(k == 0), stop=(k == 3), R=[b_oT, b_wab], W=[b_ps_y])
                gsl = slice(24 + br * 1024 + hh * 512, 24 + br * 1024 + (hh + 1) * 512)
                if br == 0:
                    p.v("dve", "tensor_tensor", y32[:, cs], ps_y[:, :], ogb[:, gsl], ALU.mult, R=[b_ps_y, b_ogb], W=[b_y32])
                else:
                    p.v("dve", "tensor_tensor", ec[:, 0:512], ps_y[:, :], ogb[:, gsl], ALU.mult, R=[b_ps_y, b_ogb], W=[b_ec])
                    p.v("pool", "tensor_tensor", mixb[:, cs], y32[:, cs], ec[:, 0:512], ALU.add, R=[b_y32, b_ec], W=[b_mixb])
        for k in range(8):
            p.tr(ps_t[:, k, :], mixb[:, k * 128:(k + 1) * 128], ident[:, :], R=[b_mixb, b_ident], W=[b_ps_t])
        p.v("dve", "tensor_copy", oT[:, :, :], ps_t[:, :, :], R=[b_ps_t], W=[b_oT])
        for hh in range(2):
            cs = slice(hh * 512, (hh + 1) * 512)
            for k in range(8):
                p.mm(ps_c[:, cs], oT[:, k, :], wob[:, k, cs], start=(k == 0), stop=(k == 7), R=[b_oT, b_wob], W=[b_ps_c])
        p.act(y32[:, :], ps_c[:, :], AF.Square, R=[b_ps_c], W=[b_y32, b_st], accum_out=st[:, 0:1])
        p.act(st[:, 1:2], st[:, 0:1], AF.Sqrt, R=[b_st], W=[b_st], scale=1.0 / D, bias=EPS)
        p.v("dve", "reciprocal", st[:, 2:3], st[:, 1:2], R=[b_st], W=[b_st])
        p.v("dve", "scalar_tensor_tensor", y32[:, :], ps_c[:, :], st[:, 2:3], pg[:, :], ALU.mult, ALU.mult, R=[b_ps_c, b_st, b_pg, b_y32], W=[b_y32])
        p.v("pool", "tensor_tensor", y32[:, :], y32[:, :], xb[:, :], ALU.add, R=[b_y32, b_xb], W=[b_y32])
        p.dma(XMd[j], y32[:, :], "y32", R=[b_y32], eng="pool", is_out=True)
    p.emit()
    p.close()
    return nc


def const_inputs():
    half = 32
    inv_freq = (10000.0 ** (-np.arange(half, dtype=np.float32) / half)).astype(np.float32)
    invf = np.tile(inv_freq, 4).reshape(128, 1).astype(np.float32)
    return {"invf": invf, "ident": np.eye(128, dtype=np.float32)}


def gain_layout(g):
    return np.ascontiguousarray(g.reshape(8, 128).T).astype(np.float32)


def run_A(nc, x_cores, pos_cores, w_in_l, gain_l):
    cst = const_inputs()
    maps = []
    for c in range(NCORES):
        maps.append({"x": x_cores[c], "pos": pos_cores[c], "w_in": w_in_l, "gain": gain_layout(gain_l),
                     "invf": cst["invf"], "ident": cst["ident"]})
    res = run_bass_kernel_spmd(nc, maps, core_ids=list(range(NCORES)))
    return res.results


def prep_B(resA, x, lw):
    bf = resA[0]["OT"].dtype
    cst = const_inputs()
    c_ = np.arange(1024, dtype=np.float32)
    CST = np.zeros((128, 1552), np.float32)
    CST[:, 0:1024] = 16.0 * c_ + 31.0
    CST[:, 1024:1280] = 64.0 * np.arange(256, dtype=np.float32)
    CST[:, 1280] = 100.0
    CST[:, 1536:1544] = 16.0 * (128.0 * np.arange(8)[None, :] + np.arange(128)[:, None]) + 31.0
    kk = np.arange(128)
    MSK = np.concatenate([(kk[:, None] <= kk[None, :]), (kk[:, None] > kk[None, :])], axis=1).astype(np.float32)
    EX = np.zeros((128, 64, 128), np.float32)
    for m in range(64):
        EX[2 * m, m, 0:64] = 1.0
        EX[2 * m + 1, m, 64:128] = 1.0
    EX = EX.reshape(128, 8192).astype(bf)
    w1 = lw["cmp_w1"]
    W1 = np.stack([np.tile(w1[kv].reshape(32, 64, 128).transpose(1, 0, 2).reshape(64, 4096), (2, 1)) for kv in range(2)]).astype(np.float32)
    POST = np.concatenate([lw["cmp_pos_emb"][kv].T for kv in range(2)], axis=1).astype(np.float32)
    B1 = np.ascontiguousarray(lw["cmp_b1"].T).astype(np.float32)
    W2 = np.concatenate([lw["cmp_w2"][0], lw["cmp_w2"][1]], axis=1).astype(np.float32)
    maps = []
    for b in range(2):
        OT = np.concatenate([resA[4 * b + r]["OT"] for r in range(4)], axis=2)
        OV = np.concatenate([resA[4 * b + r]["OV"] for r in range(4)], axis=0).reshape(SEQ, 3, 130)
        OG = np.concatenate([resA[4 * b + r]["OG"] for r in range(4)], axis=0)

        def qlay(base):
            return OT[base:base + 4].reshape(2, 4, 64, SEQ).transpose(0, 2, 1, 3).reshape(128, 4, SEQ)
        Q3 = np.stack([qlay(0), qlay(5), qlay(9)], axis=1)
        KA, KS, KW = OT[4], OT[15], OT[16]
        pad = np.zeros((128, 16), bf)
        KCBF = np.concatenate([OT[13], pad], axis=1)
        VCBF = np.concatenate([OT[14], pad], axis=1)
        VSF = np.ascontiguousarray(OV[:, 1].reshape(128, 128, 130).transpose(1, 0, 2)).reshape(128, 128 * 130)
        KSF = np.ascontiguousarray(KS)
        for r in range(4):
            QQ = np.zeros((NB, 128, 3, 4, 128), bf)
            KC = np.zeros((NB, 128, 8, 128), bf)
            VC = np.zeros((NB, 128, 8, 130), bf)
            OGB = np.zeros((NB, 128, 2072), np.float32)
            XB = np.zeros((NB, 128, D), np.float32)
            for j in range(NB):
                i = 4 * j + r
                tok = slice(i * 128, (i + 1) * 128)
                QQ[j] = Q3[:, :, :, tok]
                srcs = [(KA, 0, i - 1), (KA, 0, i)] + [(KW, 2, i - 4 + m) for m in range(5)] + [(KS, 1, i)]
                for t, (ksrc, vi, blk) in enumerate(srcs):
                    if blk >= 0:
                        KC[j, :, t, :] = ksrc[:, blk * 128:(blk + 1) * 128]
                        VC[j, :, t, :] = OV[blk * 128:(blk + 1) * 128, vi]
                OGB[j] = OG[tok]
                XB[j] = x[b, tok]
            tq = (128.0 * (4 * np.arange(NB)[None, :] + r) + np.arange(128)[:, None]).astype(np.float32)
            t0 = np.broadcast_to(128.0 * (4 * np.arange(NB)[None, :] + r), (128, NB)).astype(np.float32)
            TQC = np.concatenate([tq, tq - 128.0, t0], axis=1).astype(np.float32)
            maps.append({"QQ": QQ.reshape(NB, 128, 1536), "KC": KC.reshape(NB, 128, 1024), "VC": VC.reshape(NB, 128, 1040),
                         "KSF": KSF, "VSF": VSF, "KCBF": KCBF, "VCBF": VCBF, "OGB": OGB, "XB": XB, "TQC": TQC,
                         "TQR": np.ascontiguousarray(tq.T), "CST": CST, "MSK": MSK, "EX": EX, "W1": W1, "POST": POST, "B1": B1, "W2": W2,
                         "WA": lw["w_branch_a"], "WB": lw["w_branch_b"], "WO": lw["w_out"],
                         "PG": lw["attn_post_gain"].reshape(1, D).astype(np.float32), "SINK": lw["attn_sinks"].reshape(1, 8).astype(np.float32),
                         "ident": cst["ident"]})
    return maps


_PROGS = {}


def _prog(name, fn):
    if name not in _PROGS:
        _PROGS[name] = fn()
    return _PROGS[name]


def kernel(**inputs):
    inp = {k: np.asarray(v) for k, v in inputs.items()}
    x = np.ascontiguousarray(inp["x"], dtype=np.float32)
    pos = inp["positions"].astype(np.int32)
    pc = [np.ascontiguousarray(pos[c // 4, (c % 4) * TOK:(c % 4 + 1) * TOK]).reshape(1, TOK) for c in range(NCORES)]
    wnames = ["attn_pre_gain", "attn_post_gain", "ffn_pre_gain", "ffn_post_gain", "w_in", "attn_sinks", "cmp_pos_emb", "cmp_w1",
              "cmp_b1", "cmp_w2", "w_branch_a", "w_branch_b", "w_out", "ffn_w_up", "ffn_conv_w", "ffn_conv_b", "ffn_w_down"]
    for l in range(4):
        lw = {k: np.ascontiguousarray(inp[k][l], dtype=np.float32) for k in wnames}
        xc = [np.ascontiguousarray(x[c // 4, (c % 4) * TOK:(c % 4 + 1) * TOK]) for c in range(NCORES)]
        resA = run_A(_prog("A", build_A), xc, pc, lw["w_in"], lw["attn_pre_gain"])
        maps = prep_B(resA, x, lw)
        resB = run_bass_kernel_spmd(_prog("B", build_B), maps, core_ids=list(range(NCORES))).results
        xm = np.zeros_like(x)
        for c in range(NCORES):
            b, r = c // 4, c % 4
            xm[b].reshape(128, 128, D)[r::4] = resB[c]["XM"]
        xmc = [np.ascontiguousarray(xm[c // 4, (c % 4) * TOK:(c % 4 + 1) * TOK]) for c in range(NCORES)]
        hc = [np.ascontiguousarray(xm[c // 4, (c % 4) * TOK - 2:(c % 4) * TOK]) if c % 4 else np.zeros((2, D), np.float32)
              for c in range(NCORES)]
        resC = run_C(_prog("C", build_C), xmc, hc, lw["ffn_w_up"], lw["ffn_w_down"], lw["ffn_conv_w"], lw["ffn_conv_b"],
                     lw["ffn_pre_gain"], lw["ffn_post_gain"])
        x = np.stack([np.concatenate([resC[4 * b + r]["xo"] for r in range(4)], axis=0) for b in range(2)]).astype(np.float32)
    return x
```
